# Optimizing a Trainium2 kernel written in Bass

```python
import jax, jax.numpy as jnp
from jax import lax
import numpy as np

D_MODEL = 4096
BATCH = 2
SEQ = 8192
DEPTH = 2

D_MIX = D_MODEL
GROUP_DIM = 128
D_SC = D_MIX // 2
D_CF = D_MIX - D_SC
N_SC_GROUPS = D_SC // GROUP_DIM
N_CF_GROUPS = D_CF // GROUP_DIM
SC_WIDTH = 3
CF_WIDTH = 31
D_IN = 3 * D_SC + 2 * D_CF
IN_SPLITS = (D_SC, 2 * D_SC, 3 * D_SC, 3 * D_SC + D_CF)
D_FF = 11008
N_EXPERTS = 8
TOP_K = 2
D_FF_EXPERT = 4096
MOE_BLOCK = 512
N_MOD = 6
N_DENSE = (DEPTH + 1) // 2
N_MOE = DEPTH // 2
RMS_EPS = 1e-6
LN_EPS = 1e-5

kernel_name = "hybrid_conv_conformer_moe_adaln_block"


def rms_norm(x, g):
    xf = x.astype(jnp.float32)
    y = xf * lax.rsqrt(jnp.mean(xf * xf, axis=-1, keepdims=True) + RMS_EPS)
    return (y * g.astype(jnp.float32)).astype(x.dtype)


def layer_norm(x, g, b):
    xf = x.astype(jnp.float32)
    mu = jnp.mean(xf, axis=-1, keepdims=True)
    xc = xf - mu
    var = jnp.mean(xc * xc, axis=-1, keepdims=True)
    y = xc * lax.rsqrt(var + LN_EPS) * g.astype(jnp.float32) + b.astype(jnp.float32)
    return y.astype(x.dtype)


def causal_depthwise_conv(x, w):
    k = w.shape[0]
    return lax.conv_general_dilated(
        x, w.astype(x.dtype)[:, None, :], window_strides=(1,),
        padding=[(k - 1, 0)], dimension_numbers=("NWC", "WIO", "NWC"),
        feature_group_count=x.shape[-1])


def modulate(h, shift, scale):
    return h * (1.0 + scale) + shift


def hybrid_mixer(h, w_in, w_out, sc_w, cf_w, cf_b, cf_g, cf_beta):
    z = jnp.einsum("bsd,de->bse", h, w_in)
    b_gate, c_gate, v, a, g = jnp.split(z, IN_SPLITS, axis=-1)
    y_sc = b_gate * causal_depthwise_conv(c_gate * v, sc_w)
    u = a * jax.nn.sigmoid(g)
    u = causal_depthwise_conv(u, cf_w) + cf_b
    y_cf = jax.nn.silu(layer_norm(u, cf_g, cf_beta))
    y = jnp.concatenate([y_sc, y_cf], axis=-1)
    return jnp.einsum("bse,ed->bsd", y, w_out)


def swiglu(h, w_gate, w_up, w_down):
    a = jnp.einsum("bsd,df->bsf", h, w_gate)
    b = jnp.einsum("bsd,df->bsf", h, w_up)
    return jnp.einsum("bsf,fd->bsd", jax.nn.silu(a) * b, w_down)


def moe_swiglu(h, router_w, w_gate, w_up, w_down):
    bsz, seq, d = h.shape
    t = bsz * seq
    xt = h.reshape(t, d)
    logits = jnp.einsum("td,de->te", xt, router_w).astype(jnp.float32)
    top_vals, top_idx = lax.top_k(logits, TOP_K)
    gates = jax.nn.softmax(top_vals, axis=-1)
    n_assign = t * TOP_K
    e_flat = top_idx.reshape(-1).astype(jnp.int32)
    tok_flat = jnp.repeat(jnp.arange(t, dtype=jnp.int32), TOP_K)
    g_flat = gates.reshape(-1)
    order = jnp.argsort(e_flat)
    e_sorted = e_flat[order]
    tok_sorted = tok_flat[order]
    g_sorted = g_flat[order]
    counts = jnp.bincount(e_flat, length=N_EXPERTS).astype(jnp.int32)
    padded = (counts + MOE_BLOCK - 1) // MOE_BLOCK * MOE_BLOCK
    start = jnp.cumsum(counts) - counts
    pend = jnp.cumsum(padded)
    pstart = pend - padded
    rank = jnp.arange(n_assign, dtype=jnp.int32) - start[e_sorted]
    dest = pstart[e_sorted] + rank
    n_blocks = -(-n_assign // MOE_BLOCK) + N_EXPERTS
    cap = n_blocks * MOE_BLOCK
    tok_buf = jnp.full((cap,), t, jnp.int32).at[dest].set(tok_sorted)
    gate_buf = jnp.zeros((cap,), jnp.float32).at[dest].set(g_sorted)
    block_start = jnp.arange(n_blocks, dtype=jnp.int32) * MOE_BLOCK
    block_expert = jnp.minimum(
        jnp.searchsorted(pend, block_start, side="right"), N_EXPERTS - 1).astype(jnp.int32)
    x_pad = jnp.concatenate([xt, jnp.zeros((1, d), xt.dtype)], axis=0)

    def expert_block(args):
        e, tok = args
        xb = x_pad[tok]
        hb = jax.nn.silu(xb @ w_gate[e]) * (xb @ w_up[e])
        return hb @ w_down[e]

    y_buf = lax.map(expert_block, (block_expert, tok_buf.reshape(n_blocks, MOE_BLOCK)))
    y_buf = y_buf.reshape(cap, d) * gate_buf[:, None].astype(y_buf.dtype)
    out = jnp.zeros((t + 1, d), y_buf.dtype).at[tok_buf].add(y_buf)[:t]
    return out.reshape(bsz, seq, d)


def setup_inputs(seed: int = 0) -> dict:
    key = jax.random.key(seed)
    ks = jax.random.split(key, 24)
    f32 = jnp.float32

    def nrm(k, shape, scale):
        return jax.random.normal(k, shape, f32) * scale

    D = D_MODEL
    return {
        "x": nrm(ks[0], (BATCH, SEQ, D), 1.0),
        "c": nrm(ks[1], (BATCH, D), 1.0),
        "norm_mix_g": 1.0 + nrm(ks[2], (DEPTH, D), 0.05),
        "norm_ffn_g": 1.0 + nrm(ks[3], (DEPTH, D), 0.05),
        "w_ada": nrm(ks[4], (DEPTH, D, N_MOD * D), 0.5 * D ** -0.5),
        "b_ada": nrm(ks[5], (DEPTH, N_MOD * D), 0.02),
        "w_in": nrm(ks[6], (DEPTH, D, D_IN), D ** -0.5),
        "w_out": nrm(ks[7], (DEPTH, D_MIX, D), D_MIX ** -0.5),
        "sc_conv_w": nrm(ks[8], (DEPTH, SC_WIDTH, D_SC), SC_WIDTH ** -0.5),
        "cf_conv_w": nrm(ks[9], (DEPTH, CF_WIDTH, D_CF), CF_WIDTH ** -0.5),
        "cf_conv_b": nrm(ks[10], (DEPTH, D_CF), 0.02),
        "cf_ln_g": 1.0 + nrm(ks[11], (DEPTH, D_CF), 0.05),
        "cf_ln_b": nrm(ks[12], (DEPTH, D_CF), 0.02),
        "ffn_w_gate": nrm(ks[13], (N_DENSE, D, D_FF), D ** -0.5),
        "ffn_w_up": nrm(ks[14], (N_DENSE, D, D_FF), D ** -0.5),
        "ffn_w_down": nrm(ks[15], (N_DENSE, D_FF, D), D_FF ** -0.5),
        "router_w": nrm(ks[16], (N_MOE, D, N_EXPERTS), D ** -0.5),
        "moe_w_gate": nrm(ks[17], (N_MOE, N_EXPERTS, D, D_FF_EXPERT), D ** -0.5),
        "moe_w_up": nrm(ks[18], (N_MOE, N_EXPERTS, D, D_FF_EXPERT), D ** -0.5),
        "moe_w_down": nrm(ks[19], (N_MOE, N_EXPERTS, D_FF_EXPERT, D), D_FF_EXPERT ** -0.5),
        "final_g": 1.0 + nrm(ks[20], (D,), 0.05),
    }


def reference(x, c, norm_mix_g, norm_ffn_g, w_ada, b_ada, w_in, w_out, sc_conv_w,
              cf_conv_w, cf_conv_b, cf_ln_g, cf_ln_b, ffn_w_gate, ffn_w_up, ffn_w_down,
              router_w, moe_w_gate, moe_w_up, moe_w_down, final_g):
    c_act = jax.nn.silu(c)
    for l in range(DEPTH):
        mod = jnp.einsum("bd,de->be", c_act, w_ada[l]) + b_ada[l]
        sh1, sc1, g1, sh2, sc2, g2 = jnp.split(mod[:, None, :], N_MOD, axis=-1)
        h = modulate(rms_norm(x, norm_mix_g[l]), sh1, sc1)
        x = x + g1 * hybrid_mixer(h, w_in[l], w_out[l], sc_conv_w[l], cf_conv_w[l],
                                  cf_conv_b[l], cf_ln_g[l], cf_ln_b[l])
        h = modulate(rms_norm(x, norm_ffn_g[l]), sh2, sc2)
        if l % 2 == 0:
            i = l // 2
            f = swiglu(h, ffn_w_gate[i], ffn_w_up[i], ffn_w_down[i])
        else:
            i = l // 2
            f = moe_swiglu(h, router_w[i], moe_w_gate[i], moe_w_up[i], moe_w_down[i])
        x = x + g2 * f
    return rms_norm(x, final_g)
```

```python
import numpy as np
from contextlib import ExitStack
import concourse.bass as bass
import concourse.mybir as mybir
from concourse.bass_utils import run_bass_kernel_spmd

F32 = mybir.dt.float32
BF16 = mybir.dt.bfloat16
AF = mybir.ActivationFunctionType
ALU = mybir.AluOpType
AX = mybir.AxisListType

SEM_EPOCH = 30000


class Buf:
    __slots__ = ("name", "last_write", "reads")

    def __init__(self, name=""):
        self.name = name
        self.last_write = None
        self.reads = []


class Prog:
    ENG = ("pe", "act", "dve", "pool", "sp")

    def __init__(self, nc, ndma_sems=6):
        self.nc = nc
        self.ops = {e: [] for e in self.ENG}
        self.sems = []
        self._sem_ctx = []
        self.cur = {}
        self.waited = {e: {} for e in self.ENG}
        self.dma_pool = {}
        self.dma_rr = {e: 0 for e in self.ENG}
        self.ndma = ndma_sems

    def _new_sem(self, name):
        cm = self.nc.semaphore(name)
        h = cm.__enter__()
        self._sem_ctx.append(cm)
        self.sems.append(h)
        return len(self.sems) - 1

    def _compute_token(self, eng):
        st = self.cur.get(eng)
        if st is None or st[1] >= SEM_EPOCH:
            st = [self._new_sem(f"s_{eng}_{len(self.sems)}"), 0]
            self.cur[eng] = st
        st[1] += 1
        return (st[0], st[1])

    def _dma_token(self, q):
        pool = self.dma_pool.setdefault(q, [])
        if len(pool) < self.ndma:
            pool.append([self._new_sem(f"d_{q}_{len(pool)}"), 0, None])
        i = self.dma_rr[q] % self.ndma
        self.dma_rr[q] += 1
        ent = pool[i]
        prev = ent[2]
        if ent[1] >= SEM_EPOCH * 16:
            ent[0] = self._new_sem(f"d_{q}_{i}_{len(self.sems)}")
            ent[1] = 0
        ent[1] += 16
        tok = (ent[0], ent[1])
        ent[2] = tok
        return tok, prev

    def op(self, eng, emit, reads=(), writes=(), dma=False):
        deps = set()
        for b in reads:
            if b.last_write is not None:
                deps.add(b.last_write)
        for b in writes:
            if b.last_write is not None:
                deps.add(b.last_write)
            for t in b.reads:
                deps.add(t)
        if dma:
            tok, prev = self._dma_token(eng)
            if prev is not None:
                deps.add(prev)
            inc = 16
        else:
            tok = self._compute_token(eng)
            inc = 1
        own = self.cur[eng][0] if (not dma and eng in self.cur) else None
        w = self.waited[eng]
        need = {}
        for (s, v) in deps:
            if eng == "pe" and not dma and s == own:
                continue
            if need.get(s, 0) < v:
                need[s] = v
        waits = []
        for s, v in need.items():
            if w.get(s, 0) >= v:
                continue
            w[s] = v
            waits.append((s, v))
        self.ops[eng].append((waits, emit, tok, inc))
        for b in reads:
            b.reads.append(tok)
        for b in writes:
            b.last_write = tok
            b.reads = []
        return tok

    def final_wait(self, eng, toks):
        self.ops[eng].append((list(toks), None, None, 0))

    def emit_all(self):
        nc, sems, ops = self.nc, self.sems, self.ops
        with nc.Block() as block:
            def run(engname):
                def body(e):
                    for (waits, emit, tok, inc) in ops[engname]:
                        for (s, v) in waits:
                            e.wait_ge(sems[s], v)
                        if emit is not None:
                            emit(e).then_inc(sems[tok[0]], inc)
                return body
            block.sync(run("sp"))
            block.scalar(run("act"))
            block.vector(run("dve"))
            block.gpsimd(run("pool"))
            block.tensor(run("pe"))

    def close(self):
        for cm in reversed(self._sem_ctx):
            cm.__exit__(None, None, None)


class Cfg:
    def __init__(self, D=4096, DFF=11008, DFE=4096, NE=8, S=8192, BLK=512, L=2, NCPB=4):
        self.D, self.DFF, self.DFE, self.NE, self.S, self.BLK, self.L = D, DFF, DFE, NE, S, BLK, L
        self.NCPB = NCPB
        self.TM = S // NCPB
        self.T = self.TM + 128
        self.KC = D // 128
        self.GC = D // 256
        self.FC = DFF // 128
        self.FE = DFE // 128
        self.NB = self.TM // BLK
        KC, GC = self.KC, self.GC
        off = {}
        c = 0
        def add(name, n):
            nonlocal c
            off[name] = (c, n)
            c += n
        for l in range(L):
            add(f"gmix{l}", KC); add(f"gffn{l}", KC); add(f"bada{l}", 6 * KC)
            add(f"scw{l}", 3 * GC); add(f"cfw{l}", 31 * GC)
            add(f"cfb{l}", GC); add(f"cfg{l}", GC); add(f"cfbeta{l}", GC)
        add("fg", KC); add("c", KC); add("rw", KC * NE); add("hmask", 1)
        self.off = off
        self.NV = c


def build(cfg):
    D, KC, GC, FC, FE, NE, S, BLK, L, NB = cfg.D, cfg.KC, cfg.GC, cfg.FC, cfg.FE, cfg.NE, cfg.S, cfg.BLK, cfg.L, cfg.NB
    DFF, DFE = cfg.DFF, cfg.DFE
    HALO = 128
    T, TM = cfg.T, cfg.TM
    NSUB = BLK // 128
    nc = bass.Bass("TRN2", target_bir_lowering=False)
    def din(name, shape):
        return nc.dram_tensor(name, shape, F32, kind="ExternalInput").ap()
    x_in = din("x", [T, D])
    vecs_d = din("vecs", [128, cfg.NV])
    ident_d = din("ident", [128, 128])
    ones_d = din("ones", [128, 128])
    sel_d = din("sel", [NE, NE * 128])
    w_ada = din("w_ada", [L, D, 6 * D])
    w_in = din("w_in", [L, D, 5 * D // 2])
    w_out = din("w_out", [L, D, D])
    f_gate = din("ffn_w_gate", [D, DFF])
    f_up = din("ffn_w_up", [D, DFF])
    f_down = din("ffn_w_down", [DFF, D])
    m_gate = din("moe_w_gate", [NE, D, DFE])
    m_up = din("moe_w_up", [NE, D, DFE])
    m_down = din("moe_w_down", [NE, DFE, D])
    out_d = nc.dram_tensor("out", [TM, D], F32, kind="ExternalOutput").ap()
    xT = nc.dram_tensor("xT_scr", [KC, 128, T], F32).ap()

    es = ExitStack()
    P = Prog(nc)
    cnt = [0]
    def sb(shape, dt, name=None):
        cnt[0] += 1
        return es.enter_context(nc.sbuf_tensor(name or f"t{cnt[0]}", shape, dt))
    class Pool_:
        def __init__(self, n, shape, dt, name):
            self.t = [sb(shape, dt, f"{name}{i}") for i in range(n)]
            self.b = [Buf(f"{name}{i}") for i in range(n)]
            self.i = 0
        def get(self):
            k = self.i % len(self.t)
            self.i += 1
            return self.t[k], self.b[k]
    NWMAX = BLK + HALO
    ident = sb([128, 128], F32, "ident_s"); ones = sb([128, 128], F32, "ones_s")
    vecs = sb([128, cfg.NV], F32, "vecs_s"); sel = sb([NE, NE * 128], F32, "sel_s")
    epsr = sb([128, 1], F32, "epsr"); epsl = sb([128, 1], F32, "epsl")
    cb = Buf("const")
    P.op("sp", lambda e: e.dma_start(out=ident[:], in_=ident_d), writes=[cb], dma=True)
    P.op("sp", lambda e: e.dma_start(out=ones[:], in_=ones_d), writes=[cb], dma=True)
    P.op("sp", lambda e: e.dma_start(out=vecs[:], in_=vecs_d), writes=[cb], dma=True)
    P.op("sp", lambda e: e.dma_start(out=sel[:], in_=sel_d), writes=[cb], dma=True)
    P.op("dve", lambda e: e.memset(epsr[:], 1e-6), writes=[cb])
    P.op("dve", lambda e: e.memset(epsl[:], 1e-5), writes=[cb])
    def V(name, j=None, n=1):
        o, ln = cfg.off[name]
        if j is None:
            return vecs[:, o:o + ln]
        return vecs[:, o + j:o + j + n]
    pbank = [es.enter_context(nc.psum_tensor(f"ps{i}", [128, 512], F32)) for i in range(8)]
    pbuf = [Buf(f"ps{i}") for i in range(8)]
    wpool = Pool_(4, [128, KC, 128], BF16, "w")
    wdpool = Pool_(2, [128, max((FC + 3) // 4, (FE + 1) // 2), 128], BF16, "wd")
    xcp = Pool_(3, [128, NWMAX], F32, "xc")
    tmpp = Pool_(4, [128, NWMAX], F32, "tmp")
    hT = sb([128, KC, NWMAX], BF16, "hT"); hT_b = Buf("hT")
    yT = sb([128, KC, BLK], BF16, "yT"); yT_b = Buf("yT")
    SCR = sb([128, 2 * D], F32, "SCR"); scr_b = [Buf("scr0"), Buf("scr1")]
    assert GC * BLK <= 2 * D
    u2T = SCR[:, 0:GC * BLK].rearrange("p (g t) -> p g t", g=GC)
    u2_b = [scr_b[(g * BLK) // D] for g in range(GC)]
    class ScrPool:
        def __init__(self, view):
            self.i = 0; self.view = view
        def get(self):
            k = self.i % 2; self.i += 1
            return self.view(SCR[:, k * D:(k + 1) * D]), scr_b[k]
    wapool = ScrPool(lambda a: a.rearrange("p (k c) -> p k c", c=512))
    xin_p = ScrPool(lambda a: a)
    NHB = max((FC + 3) // 4, (FE + 1) // 2)
    assert NHB <= KC
    hb = yT; hb_b = yT_b
    rstd = sb([128, NWMAX], F32, "rstd"); rstd_b = Buf("rstd")
    mu = sb([128, BLK], F32, "mu"); mu_b = Buf("mu")
    Gbc = sb([128, 1, BLK], F32, "Gbc"); Gbc_b = Buf("Gbc")
    mods = [sb([128, 6 * KC], F32, f"mod{l}") for l in range(L)]; mod_b = [Buf() for _ in range(L)]
    gs = [[sb([128, KC], F32, f"gs{l}_{i}") for i in range(2)] for l in range(L)]
    cact = sb([128, KC], F32, "cact")

    class WT:
        def __init__(self, name, src, K, N):
            self.src, self.kch, self.nt = src, K // 128, N // 128
            self.dst = nc.dram_tensor("wb_" + name, [self.nt, 128, self.kch, 128], BF16).ap()
            self.bufs = [Buf(f"{name}{j}") for j in range(self.nt)]
        def prepass(self, js=None):
            for j in (range(self.nt) if js is None else js):
                P.op("pool", lambda e, j=j: e.dma_start(out=self.dst[j], in_=wtile_view(self.src, j)), writes=[self.bufs[j]], dma=True)
    W_in = [WT(f"in{l}", w_in[l], D, 5 * D // 2) for l in range(L)]
    W_out = [WT(f"out{l}", w_out[l], D, D) for l in range(L)]
    W_fg, W_fu, W_fd = WT("fg", f_gate, D, DFF), WT("fu", f_up, D, DFF), WT("fd", f_down, DFF, D)
    W_mg = [WT(f"mg{e}", m_gate[e], D, DFE) for e in range(NE)]
    W_mu = [WT(f"mu{e}", m_up[e], D, DFE) for e in range(NE)]
    W_md = [WT(f"md{e}", m_down[e], DFE, D) for e in range(NE)]

    def wtile_view(wap, j):
        return wap.rearrange("(kc p) n -> p kc n", p=128)[:, :, j * 128:(j + 1) * 128]

    def load_w(wt, j):
        t, b = wpool.get()
        P.op("sp", lambda e: e.dma_start(out=t[:], in_=wt.dst[j]), reads=[wt.bufs[j]], writes=[b], dma=True)
        return t, b

    def ntiles(n):
        r = []
        o = 0
        while o < n:
            r.append((o, min(512, n - o)))
            o += 512
        return r

    P.op("act", lambda e: e.activation(out=cact[:], in_=V("c"), func=AF.Silu), reads=[cb], writes=[cb])
    KQ = KC // 4
    for l in range(L):
        wav = w_ada[l].rearrange("(kc p) n -> p kc n", p=128)
        for grp in range(6 * KC // 4):
            for st in range(4):
                t, b = wapool.get()
                P.op("sp", lambda e, t=t, st=st, grp=grp, wav=wav: e.dma_start(out=t[:, 0:KQ, :], in_=wav[:, st * KQ:(st + 1) * KQ, grp * 512:(grp + 1) * 512]), writes=[b], dma=True)
                def mm(e, t=t, st=st):
                    for c in range(4):
                        for kq in range(KQ):
                            ins = e.matmul(pbank[c][:, 0:1], lhsT=t[:, kq, c * 128:(c + 1) * 128], rhs=cact[:, st * KQ + kq:st * KQ + kq + 1],
                                           start=(st == 0 and kq == 0), stop=(st == 3 and kq == KQ - 1))
                    return ins
                P.op("pe", mm, reads=[b, cb], writes=[pbuf[c] for c in range(4)])
            for c in range(4):
                j = grp * 4 + c
                P.op("dve", lambda e, l=l, j=j, c=c: e.tensor_tensor(out=mods[l][:, j:j + 1], in0=pbank[c][:, 0:1], in1=V(f"bada{l}", j), op=ALU.add),
                     reads=[pbuf[c], cb], writes=[mod_b[l]])
        P.op("dve", lambda e, l=l: e.scalar_tensor_tensor(out=gs[l][0][:], in0=mods[l][:, KC:2 * KC], scalar=1.0, in1=V(f"gmix{l}"), op0=ALU.add, op1=ALU.mult),
             reads=[mod_b[l], cb], writes=[mod_b[l]])
        P.op("dve", lambda e, l=l: e.scalar_tensor_tensor(out=gs[l][1][:], in0=mods[l][:, 4 * KC:5 * KC], scalar=1.0, in1=V(f"gffn{l}"), op0=ALU.add, op1=ALU.mult),
             reads=[mod_b[l], cb], writes=[mod_b[l]])

    for l in range(L):
        W_in[l].prepass(); W_out[l].prepass()
        if l % 2 == 0:
            for fc in range(FC):
                W_fg.prepass([fc]); W_fu.prepass([fc])
            W_fd.prepass()
        else:
            for ex in range(NE):
                for fc in range(FE):
                    W_mg[ex].prepass([fc]); W_mu[ex].prepass([fc])
                W_md[ex].prepass()
    xT_b = [Buf(f"xT{b}") for b in range(T // 128)]

    xts_p = Pool_(2, [128, 4, 128], F32, "xts")
    for tt in range(T // 128):
        blk = tt
        t, b = xin_p.get()
        P.op("act", lambda e, t=t, tt=tt: e.dma_start(out=t[:], in_=x_in[tt * 128:(tt + 1) * 128, :]), writes=[b], dma=True)
        for c4 in range(KC // 4):
            pb = c4 % 4
            def tr(e, t=t, c4=c4, pb=pb):
                for q in range(4):
                    ins = e.transpose(pbank[pb][:, q * 128:(q + 1) * 128], t[:, (c4 * 4 + q) * 128:(c4 * 4 + q + 1) * 128], ident[:])
                return ins
            P.op("pe", tr, reads=[b, cb], writes=[pbuf[pb]])
            ts_, tb_ = xts_p.get()
            eng = "act" if c4 % 2 == 0 else "dve"
            if eng == "act":
                P.op("act", lambda e, ts_=ts_, pb=pb: e.activation(out=ts_[:].rearrange("p a b -> p (a b)"), in_=pbank[pb][:], func=AF.Copy), reads=[pbuf[pb]], writes=[tb_])
            else:
                P.op("dve", lambda e, ts_=ts_, pb=pb: e.tensor_copy(out=ts_[:].rearrange("p a b -> p (a b)"), in_=pbank[pb][:]), reads=[pbuf[pb]], writes=[tb_])
            P.op("act", lambda e, ts_=ts_, c4=c4, tt=tt: e.dma_start(out=xT[c4 * 4:(c4 + 1) * 4].rearrange("c p t -> p c t")[:, :, tt * 128:(tt + 1) * 128], in_=ts_[:]),
                 reads=[tb_], writes=[xT_b[blk]], dma=True)

    def xT_bufs(c0, n):
        return [xT_b[b] for b in range(c0 // 128, (c0 + n - 1) // 128 + 1)]

    def rms_stats(c0, n, pbs):
        tl = ntiles(n)
        for kc in range(KC):
            xc, xb = xcp.get()
            P.op("act", lambda e, xc=xc, kc=kc: e.dma_start(out=xc[:, :n], in_=xT[kc, :, c0:c0 + n]), reads=xT_bufs(c0, n), writes=[xb], dma=True)
            sq, sqb = tmpp.get()
            P.op("act", lambda e, xc=xc, sq=sq: e.activation(out=sq[:, :n], in_=xc[:, :n], func=AF.Square), reads=[xb], writes=[sqb])
            def mm(e, sq=sq, kc=kc):
                for i, (o, w) in enumerate(tl):
                    ins = e.matmul(pbank[pbs[i]][:, :w], lhsT=ones[:], rhs=sq[:, o:o + w], start=(kc == 0), stop=(kc == KC - 1))
                return ins
            P.op("pe", mm, reads=[sqb, cb], writes=[pbuf[pbs[i]] for i in range(len(tl))])
        for i, (o, w) in enumerate(tl):
            P.op("act", lambda e, i=i, o=o, w=w: e.activation(out=rstd[:, o:o + w], in_=pbank[pbs[i]][:, :w], func=AF.Sqrt, scale=1.0 / D, bias=epsr[:]),
                 reads=[pbuf[pbs[i]], cb], writes=[rstd_b])
        P.op("dve", lambda e: e.reciprocal(out=rstd[:, :n], in_=rstd[:, :n]), reads=[rstd_b], writes=[rstd_b])

    def norm_mod(l, which, c0, n, router=False):
        rms_stats(c0, n, [4, 5])
        shift = mods[l][:, (0 if which == 0 else 3 * KC):]
        gsv = gs[l][which]
        for kc in range(KC):
            xc, xb = xcp.get()
            P.op("act", lambda e, xc=xc, kc=kc: e.dma_start(out=xc[:, :n], in_=xT[kc, :, c0:c0 + n]), reads=xT_bufs(c0, n), writes=[xb], dma=True)
            tm, tmb = tmpp.get()
            P.op("dve", lambda e, xc=xc, tm=tm: e.tensor_tensor(out=tm[:, :n], in0=xc[:, :n], in1=rstd[:, :n], op=ALU.mult), reads=[xb, rstd_b], writes=[tmb])
            if not router:
                P.op("act", lambda e, tm=tm, kc=kc: e.activation(out=hT[:, kc, :n], in_=tm[:, :n], func=AF.Identity, scale=gsv[:, kc:kc + 1], bias=shift[:, kc:kc + 1]),
                     reads=[tmb, mod_b[l]], writes=[hT_b])
            else:
                hf, hfb = tmpp.get()
                P.op("act", lambda e, tm=tm, kc=kc, hf=hf: e.activation(out=hf[:, :n], in_=tm[:, :n], func=AF.Identity, scale=gsv[:, kc:kc + 1], bias=shift[:, kc:kc + 1]),
                     reads=[tmb, mod_b[l]], writes=[hfb])
                P.op("dve", lambda e, kc=kc, hf=hf: e.tensor_copy(out=hT[:, kc, :n], in_=hf[:, :n]), reads=[hfb], writes=[hT_b])
                def mm(e, hf=hf, kc=kc):
                    for s in range(n // 128):
                        ins = e.matmul(pbank[s][:, 0:NE], lhsT=hf[:, s * 128:(s + 1) * 128], rhs=V("rw")[:, kc * NE:(kc + 1) * NE], start=(kc == 0), stop=(kc == KC - 1))
                    return ins
                P.op("pe", mm, reads=[hfb, cb], writes=[pbuf[s] for s in range(n // 128)])

    zc_p = Pool_(2, [128, NWMAX], F32, "zc")
    acc_p = Pool_(2, [128, BLK], F32, "acc")
    def inproj(l, j, nw, pbs, lo=0):
        t, b = load_w(W_in[l], j)
        tl = ntiles(nw)
        def mm(e, t=t):
            for i, (o, w) in enumerate(tl):
                for kc in range(KC):
                    ins = e.matmul(pbank[pbs[i]][:, :w], lhsT=t[:, kc, :], rhs=hT[:, kc, lo + o:lo + o + w], start=(kc == 0), stop=(kc == KC - 1))
            return ins
        P.op("pe", mm, reads=[b, hT_b], writes=[pbuf[pbs[i]] for i in range(len(tl))])
        return tl

    def conv(src, H, n_out, wname, l, g, K, acc):
        wv = V(wname + str(l))
        P_ = []
        for k in range(K - 1, -1, -1):
            sh = K - 1 - k
            o0 = max(0, sh - H)
            if o0 >= n_out:
                continue
            wcol = wv[:, k * GC + g:k * GC + g + 1]
            if k == K - 1:
                P_.append(lambda e, wcol=wcol: e.tensor_scalar(out=acc[:, :n_out], in0=src[:, H:H + n_out], scalar1=wcol, scalar2=None, op0=ALU.mult))
            else:
                P_.append(lambda e, wcol=wcol, o0=o0, sh=sh: e.scalar_tensor_tensor(out=acc[:, o0:n_out], in0=src[:, H + o0 - sh:H + n_out - sh], scalar=wcol,
                                                                               in1=acc[:, o0:n_out], op0=ALU.mult, op1=ALU.add))
        return P_

    def mixer(l, c0, n_out, H, mask):
        nw = n_out + H
        norm_mod(l, 0, c0 - H, nw)
        first_stat = [True]
        for g in range(GC):
            tl = inproj(l, GC + g, nw, [0, 1])
            zc, zcb = zc_p.get()
            for i, (o, w) in enumerate(tl):
                P.op("act", lambda e, i=i, o=o, w=w, zc=zc: e.activation(out=zc[:, o:o + w], in_=pbank[i][:, :w], func=AF.Copy), reads=[pbuf[i]], writes=[zcb])
            tl = inproj(l, 2 * GC + g, nw, [2, 3])
            cv, cvb = tmpp.get()
            for i, (o, w) in enumerate(tl):
                P.op("dve", lambda e, i=i, o=o, w=w, zc=zc, cv=cv: e.tensor_tensor(out=cv[:, o:o + w], in0=pbank[2 + i][:, :w], in1=zc[:, o:o + w], op=ALU.mult),
                     reads=[pbuf[2 + i], zcb], writes=[cvb])
            if mask:
                P.op("dve", lambda e, cv=cv: e.tensor_scalar(out=cv[:, 0:H], in0=cv[:, 0:H], scalar1=V("hmask"), scalar2=None, op0=ALU.mult), reads=[cvb, cb], writes=[cvb])
            acc, accb = acc_p.get()
            for f in conv(cv, H, n_out, "scw", l, g, 3, acc):
                P.op("dve", f, reads=[cvb, cb], writes=[accb])
            tl = inproj(l, g, n_out, [0], lo=H)
            P.op("dve", lambda e, acc=acc, g=g: e.tensor_tensor(out=yT[:, g, :n_out], in0=pbank[0][:, :n_out], in1=acc[:, :n_out], op=ALU.mult),
                 reads=[pbuf[0], accb], writes=[yT_b])
            tl = inproj(l, 4 * GC + g, nw, [2, 3])
            sg, sgb = zc_p.get()
            for i, (o, w) in enumerate(tl):
                P.op("act", lambda e, i=i, o=o, w=w, sg=sg: e.activation(out=sg[:, o:o + w], in_=pbank[2 + i][:, :w], func=AF.Sigmoid), reads=[pbuf[2 + i]], writes=[sgb])
            tl = inproj(l, 3 * GC + g, nw, [0, 1])
            u, ub = tmpp.get()
            for i, (o, w) in enumerate(tl):
                P.op("dve", lambda e, i=i, o=o, w=w, sg=sg, u=u: e.tensor_tensor(out=u[:, o:o + w], in0=pbank[i][:, :w], in1=sg[:, o:o + w], op=ALU.mult),
                     reads=[pbuf[i], sgb], writes=[ub])
            if mask:
                P.op("dve", lambda e, u=u: e.tensor_scalar(out=u[:, 0:H], in0=u[:, 0:H], scalar1=V("hmask"), scalar2=None, op0=ALU.mult), reads=[ub, cb], writes=[ub])
            acc, accb = acc_p.get()
            for f in conv(u, H, n_out, "cfw", l, g, 31, acc):
                P.op("dve", f, reads=[ub, cb], writes=[accb])
            P.op("act", lambda e, acc=acc, g=g: e.activation(out=u2T[:, g, :n_out], in_=acc[:, :n_out], func=AF.Identity, bias=V(f"cfb{l}", g), scale=1.0),
                 reads=[accb, cb], writes=[u2_b[g]])
            sq, sqb = tmpp.get()
            P.op("act", lambda e, sq=sq, g=g: e.activation(out=sq[:, :n_out], in_=u2T[:, g, :n_out], func=AF.Square), reads=[u2_b[g]], writes=[sqb])
            def mmst(e, g=g, sq=sq):
                e.matmul(pbank[4][:, :n_out], lhsT=ones[:], rhs=u2T[:, g, :n_out], start=(g == 0), stop=(g == GC - 1))
                return e.matmul(pbank[5][:, :n_out], lhsT=ones[:], rhs=sq[:, :n_out], start=(g == 0), stop=(g == GC - 1))
            P.op("pe", mmst, reads=[u2_b[g], sqb, cb], writes=[pbuf[4], pbuf[5]])
        DCF = D // 2
        P.op("act", lambda e: e.activation(out=mu[:, :n_out], in_=pbank[4][:, :n_out], func=AF.Copy, scale=1.0 / DCF), reads=[pbuf[4]], writes=[mu_b])
        m2, m2b = tmpp.get()
        P.op("dve", lambda e, m2=m2: e.tensor_tensor(out=m2[:, :n_out], in0=mu[:, :n_out], in1=mu[:, :n_out], op=ALU.mult), reads=[mu_b], writes=[m2b])
        P.op("dve", lambda e, m2=m2: e.scalar_tensor_tensor(out=m2[:, :n_out], in0=pbank[5][:, :n_out], scalar=1.0 / DCF, in1=m2[:, :n_out], op0=ALU.mult, op1=ALU.subtract),
             reads=[pbuf[5], m2b], writes=[m2b])
        P.op("act", lambda e, m2=m2: e.activation(out=rstd[:, :n_out], in_=m2[:, :n_out], func=AF.Sqrt, bias=epsl[:], scale=1.0), reads=[m2b, cb], writes=[rstd_b])
        P.op("dve", lambda e: e.reciprocal(out=rstd[:, :n_out], in_=rstd[:, :n_out]), reads=[rstd_b], writes=[rstd_b])
        for g in range(GC):
            t1, t1b = tmpp.get()
            P.op("dve", lambda e, t1=t1, g=g: e.tensor_tensor(out=t1[:, :n_out], in0=u2T[:, g, :n_out], in1=mu[:, :n_out], op=ALU.subtract), reads=[u2_b[g], mu_b], writes=[t1b])
            P.op("dve", lambda e, t1=t1: e.tensor_tensor(out=t1[:, :n_out], in0=t1[:, :n_out], in1=rstd[:, :n_out], op=ALU.mult), reads=[t1b, rstd_b], writes=[t1b])
            P.op("act", lambda e, t1=t1, g=g: e.activation(out=yT[:, GC + g, :n_out], in_=t1[:, :n_out], func=AF.Silu, scale=V(f"cfg{l}", g), bias=V(f"cfbeta{l}", g)),
                 reads=[t1b, cb], writes=[yT_b])
        for dc in range(KC):
            t, b = load_w(W_out[l], dc)
            pb = 6 + dc % 2
            def mm(e, t=t, pb=pb):
                for kc in range(KC):
                    ins = e.matmul(pbank[pb][:, :n_out], lhsT=t[:, kc, :], rhs=yT[:, kc, :n_out], start=(kc == 0), stop=(kc == KC - 1))
                return ins
            P.op("pe", mm, reads=[b, yT_b], writes=[pbuf[pb]])
            resid(l, 2, dc, c0, n_out, pb, None)

    def resid(l, gi, dc, c0, n, pb, gmask):
        xc, xb = xcp.get()
        P.op("act", lambda e: e.dma_start(out=xc[:, :n], in_=xT[dc, :, c0:c0 + n]), reads=xT_bufs(c0, n), writes=[xb], dma=True)
        gcol = mods[l][:, gi * KC + dc:gi * KC + dc + 1]
        if gmask is None:
            P.op("dve", lambda e: e.scalar_tensor_tensor(out=xc[:, :n], in0=pbank[pb][:, :n], scalar=gcol, in1=xc[:, :n], op0=ALU.mult, op1=ALU.add),
                 reads=[pbuf[pb], xb, mod_b[l]], writes=[xb])
        else:
            tm, tmb = tmpp.get()
            P.op("dve", lambda e: e.tensor_tensor(out=tm[:, :n], in0=pbank[pb][:, :n], in1=gmask, op=ALU.mult), reads=[pbuf[pb], Gbc_b], writes=[tmb])
            P.op("dve", lambda e: e.scalar_tensor_tensor(out=xc[:, :n], in0=tm[:, :n], scalar=gcol, in1=xc[:, :n], op0=ALU.mult, op1=ALU.add),
                 reads=[tmb, xb, mod_b[l]], writes=[xb])
        P.op("act", lambda e: e.dma_start(out=xT[dc, :, c0:c0 + n], in_=xc[:, :n]), reads=[xb], writes=xT_bufs(c0, n), dma=True)

    sa_p = Pool_(2, [128, BLK], F32, "sa")
    def ffn_part(l, c0, n, wg, wu, wd, f0, f1, gmask):
        nf = f1 - f0
        for fc in range(f0, f1):
            tg, bg = load_w(wg, fc)
            tu, bu = load_w(wu, fc)
            def mm(e, tg=tg, tu=tu):
                for kc in range(KC):
                    e.matmul(pbank[0][:, :n], lhsT=tg[:, kc, :], rhs=hT[:, kc, :n], start=(kc == 0), stop=(kc == KC - 1))
                for kc in range(KC):
                    ins = e.matmul(pbank[1][:, :n], lhsT=tu[:, kc, :], rhs=hT[:, kc, :n], start=(kc == 0), stop=(kc == KC - 1))
                return ins
            pa, pu = (0, 1) if (fc - f0) % 2 == 0 else (2, 3)
            def mm2(e, tg=tg, tu=tu, pa=pa, pu=pu):
                for kc in range(KC):
                    e.matmul(pbank[pa][:, :n], lhsT=tg[:, kc, :], rhs=hT[:, kc, :n], start=(kc == 0), stop=(kc == KC - 1))
                for kc in range(KC):
                    ins = e.matmul(pbank[pu][:, :n], lhsT=tu[:, kc, :], rhs=hT[:, kc, :n], start=(kc == 0), stop=(kc == KC - 1))
                return ins
            P.op("pe", mm2, reads=[bg, bu, hT_b], writes=[pbuf[pa], pbuf[pu]])
            sa, sab = sa_p.get()
            P.op("act", lambda e, sa=sa, pa=pa: e.activation(out=sa[:, :n], in_=pbank[pa][:, :n], func=AF.Silu), reads=[pbuf[pa]], writes=[sab])
            P.op("dve", lambda e, sa=sa, pu=pu, fc=fc: e.tensor_tensor(out=hb[:, fc - f0, :n], in0=pbank[pu][:, :n], in1=sa[:, :n], op=ALU.mult),
                 reads=[pbuf[pu], sab], writes=[hb_b])
        for dc in range(KC):
            t, b = wdpool.get()
            P.op("sp", lambda e, t=t, dc=dc: e.dma_start(out=t[:, :nf, :], in_=wd.dst[dc][:, f0:f1, :]), reads=[wd.bufs[dc]], writes=[b], dma=True)
            pb = 6 + dc % 2
            def mm(e, t=t, pb=pb):
                for k in range(nf):
                    ins = e.matmul(pbank[pb][:, :n], lhsT=t[:, k, :], rhs=hb[:, k, :n], start=(k == 0), stop=(k == nf - 1))
                return ins
            P.op("pe", mm, reads=[b, hb_b], writes=[pbuf[pb]])
            resid(l, 5, dc, c0, n, pb, gmask)

    def split(n, parts):
        q, r = divmod(n, parts)
        res, o = [], 0
        for i in range(parts):
            s = q + (1 if i < r else 0)
            if s:
                res.append((o, o + s))
            o += s
        return res

    lg = sb([128, NSUB, NE], F32, "lg"); lg2 = sb([128, NSUB, NE], F32, "lg2")
    mk1 = sb([128, NSUB, NE], F32, "mk1"); mk2 = sb([128, NSUB, NE], F32, "mk2"); G = sb([128, NSUB, NE], F32, "G")
    m1 = sb([128, NSUB], F32, "m1"); m2_ = sb([128, NSUB], F32, "m2"); g1 = sb([128, NSUB], F32, "g1"); g2 = sb([128, NSUB], F32, "g2")
    GT = sb([NE, BLK], F32, "GT")
    rt_b = Buf("router")

    def ffn(l, c0, n):
        moe = (l % 2 == 1)
        norm_mod(l, 1, c0, n, router=moe)
        if not moe:
            for (f0, f1) in split(FC, 4):
                ffn_part(l, c0, n, W_fg, W_fu, W_fd, f0, f1, None)
            return
        for s in range(NSUB):
            P.op("dve", lambda e, s=s: e.tensor_copy(out=lg[:, s, :], in_=pbank[s][:, 0:NE]), reads=[pbuf[s]], writes=[rt_b])
        P.op("dve", lambda e: e.tensor_reduce(out=m1[:], in_=lg[:], axis=AX.X, op=ALU.max), reads=[rt_b], writes=[rt_b])
        for s in range(NSUB):
            P.op("dve", lambda e, s=s: e.tensor_scalar(out=mk1[:, s, :], in0=lg[:, s, :], scalar1=m1[:, s:s + 1], scalar2=None, op0=ALU.is_equal), reads=[rt_b], writes=[rt_b])
        P.op("dve", lambda e: e.scalar_tensor_tensor(out=lg2[:], in0=mk1[:], scalar=-1e30, in1=lg[:], op0=ALU.mult, op1=ALU.add), reads=[rt_b], writes=[rt_b])
        P.op("dve", lambda e: e.tensor_reduce(out=m2_[:], in_=lg2[:], axis=AX.X, op=ALU.max), reads=[rt_b], writes=[rt_b])
        for s in range(NSUB):
            P.op("dve", lambda e, s=s: e.tensor_scalar(out=mk2[:, s, :], in0=lg2[:, s, :], scalar1=m2_[:, s:s + 1], scalar2=None, op0=ALU.is_equal), reads=[rt_b], writes=[rt_b])
        P.op("dve", lambda e: e.tensor_tensor(out=g2[:], in0=m2_[:], in1=m1[:], op=ALU.subtract), reads=[rt_b], writes=[rt_b])
        P.op("act", lambda e: e.activation(out=g2[:], in_=g2[:], func=AF.Exp), reads=[rt_b], writes=[rt_b])
        P.op("dve", lambda e: e.tensor_scalar(out=g1[:], in0=g2[:], scalar1=1.0, scalar2=None, op0=ALU.add), reads=[rt_b], writes=[rt_b])
        P.op("dve", lambda e: e.reciprocal(out=g1[:], in_=g1[:]), reads=[rt_b], writes=[rt_b])
        P.op("dve", lambda e: e.tensor_tensor(out=g2[:], in0=g2[:], in1=g1[:], op=ALU.mult), reads=[rt_b], writes=[rt_b])
        for s in range(NSUB):
            P.op("dve", lambda e, s=s: e.tensor_scalar(out=G[:, s, :], in0=mk1[:, s, :], scalar1=g1[:, s:s + 1], scalar2=None, op0=ALU.mult), reads=[rt_b], writes=[rt_b])
            P.op("dve", lambda e, s=s: e.scalar_tensor_tensor(out=G[:, s, :], in0=mk2[:, s, :], scalar=g2[:, s:s + 1], in1=G[:, s, :], op0=ALU.mult, op1=ALU.add), reads=[rt_b], writes=[rt_b])
        for s in range(NSUB):
            P.op("pe", lambda e, s=s: e.transpose(pbank[4][0:NE, s * 128:(s + 1) * 128], G[:, s, :], ident[:]), reads=[rt_b, cb], writes=[pbuf[4]])
        P.op("act", lambda e: e.activation(out=GT[:], in_=pbank[4][0:NE, :n], func=AF.Copy), reads=[pbuf[4]], writes=[rt_b])
        for ex in range(NE):
            pb = 4 + ex % 2
            P.op("pe", lambda e, ex=ex, pb=pb: e.matmul(pbank[pb][:, :n], lhsT=sel[:, ex * 128:(ex + 1) * 128], rhs=GT[:, :n], start=True, stop=True), reads=[rt_b, cb], writes=[pbuf[pb]])
            P.op("act", lambda e, ex=ex, pb=pb: e.activation(out=Gbc[:, 0, :], in_=pbank[pb][:, :n], func=AF.Copy), reads=[pbuf[pb]], writes=[Gbc_b])
            for (f0, f1) in split(FE, 2):
                ffn_part(l, c0, n, W_mg[ex], W_mu[ex], W_md[ex], f0, f1, Gbc[:, 0, :])

    def final(tt):
        c0, n = tt * 128, 128
        rms_stats(c0, n, [4])
        xn = SCR[:, 0:D].rearrange("p (k c) -> p k c", c=128)
        ot = SCR[:, D:2 * D]
        for kc in range(KC):
            xc, xb = xcp.get()
            P.op("act", lambda e, xc=xc, kc=kc: e.dma_start(out=xc[:, :n], in_=xT[kc, :, c0:c0 + n]), reads=xT_bufs(c0, n), writes=[xb], dma=True)
            P.op("dve", lambda e, xc=xc, kc=kc: e.scalar_tensor_tensor(out=xn[:, kc, :], in0=xc[:, :n], scalar=V("fg", kc), in1=rstd[:, :n], op0=ALU.mult, op1=ALU.mult),
                 reads=[xb, rstd_b, cb], writes=[scr_b[0]])
        for c4 in range(KC // 4):
            pb = c4 % 4
            def tr(e, c4=c4, pb=pb):
                for q in range(4):
                    ins = e.transpose(pbank[pb][:, q * 128:(q + 1) * 128], xn[:, c4 * 4 + q, :], ident[:])
                return ins
            P.op("pe", tr, reads=[scr_b[0], cb], writes=[pbuf[pb]])
            if c4 % 2 == 0:
                P.op("act", lambda e, c4=c4, pb=pb: e.activation(out=ot[:, c4 * 512:(c4 + 1) * 512], in_=pbank[pb][:], func=AF.Copy), reads=[pbuf[pb]], writes=[scr_b[1]])
            else:
                P.op("dve", lambda e, c4=c4, pb=pb: e.tensor_copy(out=ot[:, c4 * 512:(c4 + 1) * 512], in_=pbank[pb][:]), reads=[pbuf[pb]], writes=[scr_b[1]])
        return [P.op("act", lambda e: e.dma_start(out=out_d[c0 - HALO:c0 - HALO + 128, :], in_=ot), reads=[scr_b[1]], dma=True)]

    main = [(HALO + i * BLK, BLK) for i in range(NB)]
    for l in range(L):
        for i in reversed(range(NB)):
            c0, n = main[i]
            mixer(l, c0, n, HALO, mask=(i == 0))
        if l + 1 < L:
            mixer(l, 0, HALO, 0, mask=False)
            ffn(l, 0, HALO)
        for (c0, n) in main:
            ffn(l, c0, n)
    alltoks = []
    for tt in range(1, T // 128):
        alltoks += final(tt)
    P.final_wait("act", alltoks[-12:])
    P.emit_all()
    P.close()
    es.close()
    return nc


def _pack_inputs(cfg, b, inp, hm):
    KC, GC, NE, L = cfg.KC, cfg.GC, cfg.NE, cfg.L
    vecs = np.zeros((128, cfg.NV), np.float32)
    def put(name, arr):
        o, n = cfg.off[name]
        a = np.asarray(arr, np.float32)
        if a.ndim == 1:
            vecs[:, o:o + n] = a.reshape(-1, 128).T
        else:
            k = a.shape[0]
            vecs[:, o:o + n] = a.reshape(k, -1, 128).transpose(2, 0, 1).reshape(128, -1)
    for l in range(L):
        put(f"gmix{l}", inp["norm_mix_g"][l]); put(f"gffn{l}", inp["norm_ffn_g"][l]); put(f"bada{l}", inp["b_ada"][l])
        put(f"scw{l}", inp["sc_conv_w"][l]); put(f"cfw{l}", inp["cf_conv_w"][l])
        put(f"cfb{l}", inp["cf_conv_b"][l]); put(f"cfg{l}", inp["cf_ln_g"][l]); put(f"cfbeta{l}", inp["cf_ln_b"][l])
    put("fg", inp["final_g"]); put("c", inp["c"][b])
    o, n = cfg.off["hmask"]
    vecs[:, o] = hm
    o, n = cfg.off["rw"]
    rw = np.asarray(inp["router_w"][0], np.float32)
    vecs[:, o:o + n] = rw.reshape(KC, 128, NE).transpose(1, 0, 2).reshape(128, KC * NE)
    return vecs


_CACHE = {}


def run(cfg, inputs):
    key = (cfg.D, cfg.DFF, cfg.DFE, cfg.S, cfg.BLK, cfg.NCPB)
    if key not in _CACHE:
        _CACHE[key] = build(cfg)
    nc = _CACHE[key]
    NE = cfg.NE
    sel = np.zeros((NE, NE * 128), np.float32)
    for e in range(NE):
        sel[e, e * 128:(e + 1) * 128] = 1.0
    B = inputs["x"].shape[0]
    in_maps = []
    f = lambda a: np.ascontiguousarray(np.asarray(a, np.float32))
    shared = dict(
        ident=np.eye(128, dtype=np.float32), ones=np.ones((128, 128), np.float32), sel=sel,
        w_ada=f(inputs["w_ada"]), w_in=f(inputs["w_in"]), w_out=f(inputs["w_out"]),
        ffn_w_gate=f(inputs["ffn_w_gate"][0]), ffn_w_up=f(inputs["ffn_w_up"][0]), ffn_w_down=f(inputs["ffn_w_down"][0]),
        moe_w_gate=f(inputs["moe_w_gate"][0]), moe_w_up=f(inputs["moe_w_up"][0]), moe_w_down=f(inputs["moe_w_down"][0]),
    )
    NCPB, TM, T = cfg.NCPB, cfg.TM, cfg.T
    x = np.asarray(inputs["x"], np.float32)
    for b in range(B):
        for j in range(NCPB):
            m = dict(shared)
            xs = np.zeros((T, cfg.D), np.float32)
            lo = j * TM
            if j > 0:
                xs[:] = x[b, lo - 128:lo + TM]
            else:
                xs[128:] = x[b, 0:TM]
            m["x"] = xs
            m["vecs"] = _pack_inputs(cfg, b, inputs, 0.0 if j == 0 else 1.0)
            in_maps.append(m)
    ncores = B * NCPB
    res = run_bass_kernel_spmd(nc, in_maps, core_ids=list(range(ncores)))
    out = np.empty((B, cfg.S, cfg.D), np.float32)
    for b in range(B):
        for j in range(NCPB):
            out[b, j * TM:(j + 1) * TM] = res.results[b * NCPB + j]["out"]
    return out


def kernel(**inputs):
    cfg = Cfg()
    return run(cfg, inputs)
```

```python
import numpy as np
from contextlib import ExitStack
import concourse.bass as bass
import concourse.mybir as mybir
from concourse.bass_utils import run_bass_kernel_spmd

F32 = mybir.dt.float32
BF16 = mybir.dt.bfloat16
AF = mybir.ActivationFunctionType
ALU = mybir.AluOpType
AX = mybir.AxisListType

SEM_EPOCH = 30000


class Buf:
    __slots__ = ("name", "last_write", "reads")

    def __init__(self, name=""):
        self.name = name
        self.last_write = None
        self.reads = []


class Prog:
    ENG = ("pe", "act", "dve", "pool", "sp")

    def __init__(self, nc, ndma_sems=6):
        self.nc = nc
        self.ops = {e: [] for e in self.ENG}
        self.sems = []
        self._sem_ctx = []
        self.cur = {}
        self.waited = {e: {} for e in self.ENG}
        self.dma_pool = {}
        self.dma_rr = {e: 0 for e in self.ENG}
        self.ndma = ndma_sems

    def _new_sem(self, name):
        cm = self.nc.semaphore(name)
        h = cm.__enter__()
        self._sem_ctx.append(cm)
        self.sems.append(h)
        return len(self.sems) - 1

    def _compute_token(self, eng):
        st = self.cur.get(eng)
        if st is None or st[1] >= SEM_EPOCH:
            st = [self._new_sem(f"s_{eng}_{len(self.sems)}"), 0]
            self.cur[eng] = st
        st[1] += 1
        return (st[0], st[1])

    def _dma_token(self, q):
        pool = self.dma_pool.setdefault(q, [])
        if len(pool) < self.ndma:
            pool.append([self._new_sem(f"d_{q}_{len(pool)}"), 0, None])
        i = self.dma_rr[q] % self.ndma
        self.dma_rr[q] += 1
        ent = pool[i]
        prev = ent[2]
        if ent[1] >= SEM_EPOCH * 16:
            ent[0] = self._new_sem(f"d_{q}_{i}_{len(self.sems)}")
            ent[1] = 0
        ent[1] += 16
        tok = (ent[0], ent[1])
        ent[2] = tok
        return tok, prev

    def op(self, eng, emit, reads=(), writes=(), dma=False):
        deps = set()
        for b in reads:
            if b.last_write is not None:
                deps.add(b.last_write)
        for b in writes:
            if b.last_write is not None:
                deps.add(b.last_write)
            for t in b.reads:
                deps.add(t)
        if dma:
            tok, prev = self._dma_token(eng)
            if prev is not None:
                deps.add(prev)
            inc = 16
        else:
            tok = self._compute_token(eng)
            inc = 1
        own = self.cur[eng][0] if (not dma and eng in self.cur) else None
        w = self.waited[eng]
        need = {}
        for (s, v) in deps:
            if eng == "pe" and not dma and s == own:
                continue
            if need.get(s, 0) < v:
                need[s] = v
        waits = []
        for s, v in need.items():
            if w.get(s, 0) >= v:
                continue
            w[s] = v
            waits.append((s, v))
        self.ops[eng].append((waits, emit, tok, inc))
        for b in reads:
            b.reads.append(tok)
        for b in writes:
            b.last_write = tok
            b.reads = []
        return tok

    def final_wait(self, eng, toks):
        self.ops[eng].append((list(toks), None, None, 0))

    def emit_all(self):
        nc, sems, ops = self.nc, self.sems, self.ops
        with nc.Block() as block:
            def run(engname):
                def body(e):
                    for (waits, emit, tok, inc) in ops[engname]:
                        for (s, v) in waits:
                            e.wait_ge(sems[s], v)
                        if emit is not None:
                            emit(e).then_inc(sems[tok[0]], inc)
                return body
            block.sync(run("sp"))
            block.scalar(run("act"))
            block.vector(run("dve"))
            block.gpsimd(run("pool"))
            block.tensor(run("pe"))

    def close(self):
        for cm in reversed(self._sem_ctx):
            cm.__exit__(None, None, None)


class Cfg:
    def __init__(self, D=4096, DFF=11008, DFE=4096, NE=8, S=8192, BLK=512, L=2, NCPB=4):
        self.D, self.DFF, self.DFE, self.NE, self.S, self.BLK, self.L = D, DFF, DFE, NE, S, BLK, L
        self.NCPB = NCPB
        self.TM = S // NCPB
        self.T = self.TM + 128
        self.KC = D // 128
        self.GC = D // 256
        self.FC = DFF // 128
        self.FE = DFE // 128
        self.NB = self.TM // BLK
        KC, GC = self.KC, self.GC
        off = {}
        c = 0
        def add(name, n):
            nonlocal c
            off[name] = (c, n)
            c += n
        for l in range(L):
            add(f"gmix{l}", KC); add(f"gffn{l}", KC); add(f"bada{l}", 6 * KC)
            add(f"scw{l}", 3 * GC); add(f"cfw{l}", 31 * GC)
            add(f"cfb{l}", GC); add(f"cfg{l}", GC); add(f"cfbeta{l}", GC)
        add("fg", KC); add("c", KC); add("rw", KC * NE); add("hmask", 1)
        self.off = off
        self.NV = c


def build(cfg):
    D, KC, GC, FC, FE, NE, S, BLK, L, NB = cfg.D, cfg.KC, cfg.GC, cfg.FC, cfg.FE, cfg.NE, cfg.S, cfg.BLK, cfg.L, cfg.NB
    DFF, DFE = cfg.DFF, cfg.DFE
    HALO = 128
    T, TM = cfg.T, cfg.TM
    NSUB = BLK // 128
    nc = bass.Bass("TRN2", target_bir_lowering=False)
    def din(name, shape):
        return nc.dram_tensor(name, shape, F32, kind="ExternalInput").ap()
    x_in = din("x", [T, D])
    vecs_d = din("vecs", [128, cfg.NV])
    ident_d = din("ident", [128, 128])
    ones_d = din("ones", [128, 128])
    sel_d = din("sel", [NE, NE * 128])
    w_ada = din("w_ada", [L, D, 6 * D])
    w_in = din("w_in", [L, D, 5 * D // 2])
    w_out = din("w_out", [L, D, D])
    f_gate = din("ffn_w_gate", [D, DFF])
    f_up = din("ffn_w_up", [D, DFF])
    f_down = din("ffn_w_down", [DFF, D])
    m_gate = din("moe_w_gate", [NE, D, DFE])
    m_up = din("moe_w_up", [NE, D, DFE])
    m_down = din("moe_w_down", [NE, DFE, D])
    out_d = nc.dram_tensor("out", [TM, D], F32, kind="ExternalOutput").ap()
    xT = nc.dram_tensor("xT_scr", [KC, 128, T], F32).ap()

    es = ExitStack()
    P = Prog(nc)
    cnt = [0]
    def sb(shape, dt, name=None):
        cnt[0] += 1
        return es.enter_context(nc.sbuf_tensor(name or f"t{cnt[0]}", shape, dt))
    class Pool_:
        def __init__(self, n, shape, dt, name):
            self.t = [sb(shape, dt, f"{name}{i}") for i in range(n)]
            self.b = [Buf(f"{name}{i}") for i in range(n)]
            self.i = 0
        def get(self):
            k = self.i % len(self.t)
            self.i += 1
            return self.t[k], self.b[k]
    NWMAX = BLK + HALO
    ident = sb([128, 128], F32, "ident_s"); ones = sb([128, 128], F32, "ones_s")
    vecs = sb([128, cfg.NV], F32, "vecs_s"); sel = sb([NE, NE * 128], F32, "sel_s")
    epsr = sb([128, 1], F32, "epsr"); epsl = sb([128, 1], F32, "epsl")
    cb = Buf("const")
    P.op("sp", lambda e: e.dma_start(out=ident[:], in_=ident_d), writes=[cb], dma=True)
    P.op("sp", lambda e: e.dma_start(out=ones[:], in_=ones_d), writes=[cb], dma=True)
    P.op("sp", lambda e: e.dma_start(out=vecs[:], in_=vecs_d), writes=[cb], dma=True)
    P.op("sp", lambda e: e.dma_start(out=sel[:], in_=sel_d), writes=[cb], dma=True)
    P.op("dve", lambda e: e.memset(epsr[:], 1e-6), writes=[cb])
    P.op("dve", lambda e: e.memset(epsl[:], 1e-5), writes=[cb])
    def V(name, j=None, n=1):
        o, ln = cfg.off[name]
        if j is None:
            return vecs[:, o:o + ln]
        return vecs[:, o + j:o + j + n]
    pbank = [es.enter_context(nc.psum_tensor(f"ps{i}", [128, 512], F32)) for i in range(8)]
    pbuf = [Buf(f"ps{i}") for i in range(8)]
    wpool = Pool_(4, [128, KC, 128], BF16, "w")
    wdpool = Pool_(2, [128, max((FC + 3) // 4, (FE + 1) // 2), 128], BF16, "wd")
    xcp = Pool_(3, [128, NWMAX], F32, "xc")
    tmpp = Pool_(4, [128, NWMAX], F32, "tmp")
    hT = sb([128, KC, NWMAX], BF16, "hT"); hT_b = Buf("hT")
    yT = sb([128, KC, BLK], BF16, "yT"); yT_b = Buf("yT")
    SCR = sb([128, 2 * D], F32, "SCR"); scr_b = [Buf("scr0"), Buf("scr1")]
    assert GC * BLK <= 2 * D
    u2T = SCR[:, 0:GC * BLK].rearrange("p (g t) -> p g t", g=GC)
    u2_b = [scr_b[(g * BLK) // D] for g in range(GC)]
    class ScrPool:
        def __init__(self, view):
            self.i = 0; self.view = view
        def get(self):
            k = self.i % 2; self.i += 1
            return self.view(SCR[:, k * D:(k + 1) * D]), scr_b[k]
    wapool = ScrPool(lambda a: a.rearrange("p (k c) -> p k c", c=512))
    xin_p = ScrPool(lambda a: a)
    NHB = max((FC + 3) // 4, (FE + 1) // 2)
    assert NHB <= KC
    hb = yT; hb_b = yT_b
    rstd = sb([128, NWMAX], F32, "rstd"); rstd_b = Buf("rstd")
    mu = sb([128, BLK], F32, "mu"); mu_b = Buf("mu")
    Gbc = sb([128, 1, BLK], F32, "Gbc"); Gbc_b = Buf("Gbc")
    mods = [sb([128, 6 * KC], F32, f"mod{l}") for l in range(L)]; mod_b = [Buf() for _ in range(L)]
    gs = [[sb([128, KC], F32, f"gs{l}_{i}") for i in range(2)] for l in range(L)]
    cact = sb([128, KC], F32, "cact")

    class WT:
        def __init__(self, name, src, K, N):
            self.src, self.kch, self.nt = src, K // 128, N // 128
            self.dst = nc.dram_tensor("wb_" + name, [self.nt, 128, self.kch, 128], BF16).ap()
            self.bufs = [Buf(f"{name}{j}") for j in range(self.nt)]
        def prepass(self, js=None):
            for j in (range(self.nt) if js is None else js):
                P.op("pool", lambda e, j=j: e.dma_start(out=self.dst[j], in_=wtile_view(self.src, j)), writes=[self.bufs[j]], dma=True)
    W_in = [WT(f"in{l}", w_in[l], D, 5 * D // 2) for l in range(L)]
    W_out = [WT(f"out{l}", w_out[l], D, D) for l in range(L)]
    W_fg, W_fu, W_fd = WT("fg", f_gate, D, DFF), WT("fu", f_up, D, DFF), WT("fd", f_down, DFF, D)
    W_mg = [WT(f"mg{e}", m_gate[e], D, DFE) for e in range(NE)]
    W_mu = [WT(f"mu{e}", m_up[e], D, DFE) for e in range(NE)]
    W_md = [WT(f"md{e}", m_down[e], DFE, D) for e in range(NE)]

    def wtile_view(wap, j):
        return wap.rearrange("(kc p) n -> p kc n", p=128)[:, :, j * 128:(j + 1) * 128]

    def load_w(wt, j):
        t, b = wpool.get()
        P.op("sp", lambda e: e.dma_start(out=t[:], in_=wt.dst[j]), reads=[wt.bufs[j]], writes=[b], dma=True)
        return t, b

    def ntiles(n):
        r = []
        o = 0
        while o < n:
            r.append((o, min(512, n - o)))
            o += 512
        return r

    P.op("act", lambda e: e.activation(out=cact[:], in_=V("c"), func=AF.Silu), reads=[cb], writes=[cb])
    KQ = KC // 4
    for l in range(L):
        wav = w_ada[l].rearrange("(kc p) n -> p kc n", p=128)
        for grp in range(6 * KC // 4):
            for st in range(4):
                t, b = wapool.get()
                P.op("sp", lambda e, t=t, st=st, grp=grp, wav=wav: e.dma_start(out=t[:, 0:KQ, :], in_=wav[:, st * KQ:(st + 1) * KQ, grp * 512:(grp + 1) * 512]), writes=[b], dma=True)
                def mm(e, t=t, st=st):
                    for c in range(4):
                        for kq in range(KQ):
                            ins = e.matmul(pbank[c][:, 0:1], lhsT=t[:, kq, c * 128:(c + 1) * 128], rhs=cact[:, st * KQ + kq:st * KQ + kq + 1],
                                           start=(st == 0 and kq == 0), stop=(st == 3 and kq == KQ - 1))
                    return ins
                P.op("pe", mm, reads=[b, cb], writes=[pbuf[c] for c in range(4)])
            for c in range(4):
                j = grp * 4 + c
                P.op("dve", lambda e, l=l, j=j, c=c: e.tensor_tensor(out=mods[l][:, j:j + 1], in0=pbank[c][:, 0:1], in1=V(f"bada{l}", j), op=ALU.add),
                     reads=[pbuf[c], cb], writes=[mod_b[l]])
        P.op("dve", lambda e, l=l: e.scalar_tensor_tensor(out=gs[l][0][:], in0=mods[l][:, KC:2 * KC], scalar=1.0, in1=V(f"gmix{l}"), op0=ALU.add, op1=ALU.mult),
             reads=[mod_b[l], cb], writes=[mod_b[l]])
        P.op("dve", lambda e, l=l: e.scalar_tensor_tensor(out=gs[l][1][:], in0=mods[l][:, 4 * KC:5 * KC], scalar=1.0, in1=V(f"gffn{l}"), op0=ALU.add, op1=ALU.mult),
             reads=[mod_b[l], cb], writes=[mod_b[l]])

    for l in range(L):
        W_in[l].prepass(); W_out[l].prepass()
        if l % 2 == 0:
            for fc in range(FC):
                W_fg.prepass([fc]); W_fu.prepass([fc])
            W_fd.prepass()
        else:
            for ex in range(NE):
                for fc in range(FE):
                    W_mg[ex].prepass([fc]); W_mu[ex].prepass([fc])
                W_md[ex].prepass()
    xT_b = [Buf(f"xT{b}") for b in range(T // 128)]

    xts_p = Pool_(2, [128, 4, 128], F32, "xts")
    for tt in range(T // 128):
        blk = tt
        t, b = xin_p.get()
        P.op("act", lambda e, t=t, tt=tt: e.dma_start(out=t[:], in_=x_in[tt * 128:(tt + 1) * 128, :]), writes=[b], dma=True)
        for c4 in range(KC // 4):
            pb = c4 % 4
            def tr(e, t=t, c4=c4, pb=pb):
                for q in range(4):
                    ins = e.transpose(pbank[pb][:, q * 128:(q + 1) * 128], t[:, (c4 * 4 + q) * 128:(c4 * 4 + q + 1) * 128], ident[:])
                return ins
            P.op("pe", tr, reads=[b, cb], writes=[pbuf[pb]])
            ts_, tb_ = xts_p.get()
            eng = "act" if c4 % 2 == 0 else "dve"
            if eng == "act":
                P.op("act", lambda e, ts_=ts_, pb=pb: e.activation(out=ts_[:].rearrange("p a b -> p (a b)"), in_=pbank[pb][:], func=AF.Copy), reads=[pbuf[pb]], writes=[tb_])
            else:
                P.op("dve", lambda e, ts_=ts_, pb=pb: e.tensor_copy(out=ts_[:].rearrange("p a b -> p (a b)"), in_=pbank[pb][:]), reads=[pbuf[pb]], writes=[tb_])
            P.op("act", lambda e, ts_=ts_, c4=c4, tt=tt: e.dma_start(out=xT[c4 * 4:(c4 + 1) * 4].rearrange("c p t -> p c t")[:, :, tt * 128:(tt + 1) * 128], in_=ts_[:]),
                 reads=[tb_], writes=[xT_b[blk]], dma=True)

    def xT_bufs(c0, n):
        return [xT_b[b] for b in range(c0 // 128, (c0 + n - 1) // 128 + 1)]

    def rms_stats(c0, n, pbs):
        tl = ntiles(n)
        for kc in range(KC):
            xc, xb = xcp.get()
            P.op("act", lambda e, xc=xc, kc=kc: e.dma_start(out=xc[:, :n], in_=xT[kc, :, c0:c0 + n]), reads=xT_bufs(c0, n), writes=[xb], dma=True)
            sq, sqb = tmpp.get()
            P.op("act", lambda e, xc=xc, sq=sq: e.activation(out=sq[:, :n], in_=xc[:, :n], func=AF.Square), reads=[xb], writes=[sqb])
            def mm(e, sq=sq, kc=kc):
                for i, (o, w) in enumerate(tl):
                    ins = e.matmul(pbank[pbs[i]][:, :w], lhsT=ones[:], rhs=sq[:, o:o + w], start=(kc == 0), stop=(kc == KC - 1))
                return ins
            P.op("pe", mm, reads=[sqb, cb], writes=[pbuf[pbs[i]] for i in range(len(tl))])
        for i, (o, w) in enumerate(tl):
            P.op("act", lambda e, i=i, o=o, w=w: e.activation(out=rstd[:, o:o + w], in_=pbank[pbs[i]][:, :w], func=AF.Sqrt, scale=1.0 / D, bias=epsr[:]),
                 reads=[pbuf[pbs[i]], cb], writes=[rstd_b])
        P.op("dve", lambda e: e.reciprocal(out=rstd[:, :n], in_=rstd[:, :n]), reads=[rstd_b], writes=[rstd_b])

    def norm_mod(l, which, c0, n, router=False):
        rms_stats(c0, n, [4, 5])
        shift = mods[l][:, (0 if which == 0 else 3 * KC):]
        gsv = gs[l][which]
        for kc in range(KC):
            xc, xb = xcp.get()
            P.op("act", lambda e, xc=xc, kc=kc: e.dma_start(out=xc[:, :n], in_=xT[kc, :, c0:c0 + n]), reads=xT_bufs(c0, n), writes=[xb], dma=True)
            tm, tmb = tmpp.get()
            P.op("dve", lambda e, xc=xc, tm=tm: e.tensor_tensor(out=tm[:, :n], in0=xc[:, :n], in1=rstd[:, :n], op=ALU.mult), reads=[xb, rstd_b], writes=[tmb])
            if not router:
                P.op("act", lambda e, tm=tm, kc=kc: e.activation(out=hT[:, kc, :n], in_=tm[:, :n], func=AF.Identity, scale=gsv[:, kc:kc + 1], bias=shift[:, kc:kc + 1]),
                     reads=[tmb, mod_b[l]], writes=[hT_b])
            else:
                hf, hfb = tmpp.get()
                P.op("act", lambda e, tm=tm, kc=kc, hf=hf: e.activation(out=hf[:, :n], in_=tm[:, :n], func=AF.Identity, scale=gsv[:, kc:kc + 1], bias=shift[:, kc:kc + 1]),
                     reads=[tmb, mod_b[l]], writes=[hfb])
                P.op("dve", lambda e, kc=kc, hf=hf: e.tensor_copy(out=hT[:, kc, :n], in_=hf[:, :n]), reads=[hfb], writes=[hT_b])
                def mm(e, hf=hf, kc=kc):
                    for s in range(n // 128):
                        ins = e.matmul(pbank[s][:, 0:NE], lhsT=hf[:, s * 128:(s + 1) * 128], rhs=V("rw")[:, kc * NE:(kc + 1) * NE], start=(kc == 0), stop=(kc == KC - 1))
                    return ins
                P.op("pe", mm, reads=[hfb, cb], writes=[pbuf[s] for s in range(n // 128)])

    zc_p = Pool_(2, [128, NWMAX], F32, "zc")
    acc_p = Pool_(2, [128, BLK], F32, "acc")
    def inproj(l, j, nw, pbs, lo=0):
        t, b = load_w(W_in[l], j)
        tl = ntiles(nw)
        def mm(e, t=t):
            for i, (o, w) in enumerate(tl):
                for kc in range(KC):
                    ins = e.matmul(pbank[pbs[i]][:, :w], lhsT=t[:, kc, :], rhs=hT[:, kc, lo + o:lo + o + w], start=(kc == 0), stop=(kc == KC - 1))
            return ins
        P.op("pe", mm, reads=[b, hT_b], writes=[pbuf[pbs[i]] for i in range(len(tl))])
        return tl

    def conv(src, H, n_out, wname, l, g, K, acc):
        wv = V(wname + str(l))
        P_ = []
        for k in range(K - 1, -1, -1):
            sh = K - 1 - k
            o0 = max(0, sh - H)
            if o0 >= n_out:
                continue
            wcol = wv[:, k * GC + g:k * GC + g + 1]
            if k == K - 1:
                P_.append(lambda e, wcol=wcol: e.tensor_scalar(out=acc[:, :n_out], in0=src[:, H:H + n_out], scalar1=wcol, scalar2=None, op0=ALU.mult))
            else:
                P_.append(lambda e, wcol=wcol, o0=o0, sh=sh: e.scalar_tensor_tensor(out=acc[:, o0:n_out], in0=src[:, H + o0 - sh:H + n_out - sh], scalar=wcol,
                                                                               in1=acc[:, o0:n_out], op0=ALU.mult, op1=ALU.add))
        return P_

    def mixer(l, c0, n_out, H, mask):
        nw = n_out + H
        norm_mod(l, 0, c0 - H, nw)
        pend_stats = []
        for g in range(GC):
            tl = inproj(l, GC + g, nw, [0, 1])
            zc, zcb = zc_p.get()
            for i, (o, w) in enumerate(tl):
                P.op("act", lambda e, i=i, o=o, w=w, zc=zc: e.activation(out=zc[:, o:o + w], in_=pbank[i][:, :w], func=AF.Copy), reads=[pbuf[i]], writes=[zcb])
            tl = inproj(l, 2 * GC + g, nw, [2, 3])
            cv, cvb = tmpp.get()
            for i, (o, w) in enumerate(tl):
                P.op("dve", lambda e, i=i, o=o, w=w, zc=zc, cv=cv: e.tensor_tensor(out=cv[:, o:o + w], in0=pbank[2 + i][:, :w], in1=zc[:, o:o + w], op=ALU.mult),
                     reads=[pbuf[2 + i], zcb], writes=[cvb])
            if mask:
                P.op("dve", lambda e, cv=cv: e.tensor_scalar(out=cv[:, 0:H], in0=cv[:, 0:H], scalar1=V("hmask"), scalar2=None, op0=ALU.mult), reads=[cvb, cb], writes=[cvb])
            acc, accb = acc_p.get()
            for f in conv(cv, H, n_out, "scw", l, g, 3, acc):
                P.op("dve", f, reads=[cvb, cb], writes=[accb])
            tl = inproj(l, g, n_out, [0], lo=H)
            P.op("dve", lambda e, acc=acc, g=g: e.tensor_tensor(out=yT[:, g, :n_out], in0=pbank[0][:, :n_out], in1=acc[:, :n_out], op=ALU.mult),
                 reads=[pbuf[0], accb], writes=[yT_b])
            tl = inproj(l, 4 * GC + g, nw, [2, 3])
            sg, sgb = zc_p.get()
            for i, (o, w) in enumerate(tl):
                P.op("act", lambda e, i=i, o=o, w=w, sg=sg: e.activation(out=sg[:, o:o + w], in_=pbank[2 + i][:, :w], func=AF.Sigmoid), reads=[pbuf[2 + i]], writes=[sgb])
            tl = inproj(l, 3 * GC + g, nw, [0, 1])
            u, ub = tmpp.get()
            for i, (o, w) in enumerate(tl):
                P.op("dve", lambda e, i=i, o=o, w=w, sg=sg, u=u: e.tensor_tensor(out=u[:, o:o + w], in0=pbank[i][:, :w], in1=sg[:, o:o + w], op=ALU.mult),
                     reads=[pbuf[i], sgb], writes=[ub])
            if mask:
                P.op("dve", lambda e, u=u: e.tensor_scalar(out=u[:, 0:H], in0=u[:, 0:H], scalar1=V("hmask"), scalar2=None, op0=ALU.mult), reads=[ub, cb], writes=[ub])
            acc, accb = acc_p.get()
            for f in conv(u, H, n_out, "cfw", l, g, 31, acc):
                P.op("dve", f, reads=[ub, cb], writes=[accb])
            P.op("act", lambda e, acc=acc, g=g: e.activation(out=u2T[:, g, :n_out], in_=acc[:, :n_out], func=AF.Identity, bias=V(f"cfb{l}", g), scale=1.0),
                 reads=[accb, cb], writes=[u2_b[g]])
            sq, sqb = tmpp.get()
            P.op("act", lambda e, sq=sq, g=g: e.activation(out=sq[:, :n_out], in_=u2T[:, g, :n_out], func=AF.Square), reads=[u2_b[g]], writes=[sqb])
            def mmst(e, g=g, sq=sq):
                e.matmul(pbank[4][:, :n_out], lhsT=ones[:], rhs=u2T[:, g, :n_out], start=(g == 0), stop=(g == GC - 1))
                return e.matmul(pbank[5][:, :n_out], lhsT=ones[:], rhs=sq[:, :n_out], start=(g == 0), stop=(g == GC - 1))
            pend_stats.append((mmst, [u2_b[g], sqb, cb]))
            if len(pend_stats) > 1:
                f_, r_ = pend_stats.pop(0)
                P.op("pe", f_, reads=r_, writes=[pbuf[4], pbuf[5]])
        for f_, r_ in pend_stats:
            P.op("pe", f_, reads=r_, writes=[pbuf[4], pbuf[5]])
        DCF = D // 2
        P.op("act", lambda e: e.activation(out=mu[:, :n_out], in_=pbank[4][:, :n_out], func=AF.Copy, scale=1.0 / DCF), reads=[pbuf[4]], writes=[mu_b])
        m2, m2b = tmpp.get()
        P.op("dve", lambda e, m2=m2: e.tensor_tensor(out=m2[:, :n_out], in0=mu[:, :n_out], in1=mu[:, :n_out], op=ALU.mult), reads=[mu_b], writes=[m2b])
        P.op("dve", lambda e, m2=m2: e.scalar_tensor_tensor(out=m2[:, :n_out], in0=pbank[5][:, :n_out], scalar=1.0 / DCF, in1=m2[:, :n_out], op0=ALU.mult, op1=ALU.subtract),
             reads=[pbuf[5], m2b], writes=[m2b])
        P.op("act", lambda e, m2=m2: e.activation(out=rstd[:, :n_out], in_=m2[:, :n_out], func=AF.Sqrt, bias=epsl[:], scale=1.0), reads=[m2b, cb], writes=[rstd_b])
        P.op("dve", lambda e: e.reciprocal(out=rstd[:, :n_out], in_=rstd[:, :n_out]), reads=[rstd_b], writes=[rstd_b])
        for g in range(GC):
            t1, t1b = tmpp.get()
            P.op("dve", lambda e, t1=t1, g=g: e.tensor_tensor(out=t1[:, :n_out], in0=u2T[:, g, :n_out], in1=mu[:, :n_out], op=ALU.subtract), reads=[u2_b[g], mu_b], writes=[t1b])
            P.op("dve", lambda e, t1=t1: e.tensor_tensor(out=t1[:, :n_out], in0=t1[:, :n_out], in1=rstd[:, :n_out], op=ALU.mult), reads=[t1b, rstd_b], writes=[t1b])
            P.op("act", lambda e, t1=t1, g=g: e.activation(out=yT[:, GC + g, :n_out], in_=t1[:, :n_out], func=AF.Silu, scale=V(f"cfg{l}", g), bias=V(f"cfbeta{l}", g)),
                 reads=[t1b, cb], writes=[yT_b])
        pre = resid_load(0, c0, n_out)
        for dc in range(KC):
            t, b = load_w(W_out[l], dc)
            pb = 6 + dc % 2
            def mm(e, t=t, pb=pb):
                for kc in range(KC):
                    ins = e.matmul(pbank[pb][:, :n_out], lhsT=t[:, kc, :], rhs=yT[:, kc, :n_out], start=(kc == 0), stop=(kc == KC - 1))
                return ins
            P.op("pe", mm, reads=[b, yT_b], writes=[pbuf[pb]])
            nxt = resid_load(dc + 1, c0, n_out) if dc + 1 < KC else None
            resid_fin(l, 2, dc, c0, n_out, pb, None, pre)
            pre = nxt

    def resid_load(dc, c0, n):
        xc, xb = xcp.get()
        P.op("act", lambda e: e.dma_start(out=xc[:, :n], in_=xT[dc, :, c0:c0 + n]), reads=xT_bufs(c0, n), writes=[xb], dma=True)
        return xc, xb

    def resid_fin(l, gi, dc, c0, n, pb, gmask, pre):
        xc, xb = pre
        gcol = mods[l][:, gi * KC + dc:gi * KC + dc + 1]
        if gmask is None:
            P.op("dve", lambda e: e.scalar_tensor_tensor(out=xc[:, :n], in0=pbank[pb][:, :n], scalar=gcol, in1=xc[:, :n], op0=ALU.mult, op1=ALU.add),
                 reads=[pbuf[pb], xb, mod_b[l]], writes=[xb])
        else:
            tm, tmb = tmpp.get()
            P.op("dve", lambda e: e.tensor_tensor(out=tm[:, :n], in0=pbank[pb][:, :n], in1=gmask, op=ALU.mult), reads=[pbuf[pb], Gbc_b], writes=[tmb])
            P.op("dve", lambda e: e.scalar_tensor_tensor(out=xc[:, :n], in0=tm[:, :n], scalar=gcol, in1=xc[:, :n], op0=ALU.mult, op1=ALU.add),
                 reads=[tmb, xb, mod_b[l]], writes=[xb])
        P.op("act", lambda e: e.dma_start(out=xT[dc, :, c0:c0 + n], in_=xc[:, :n]), reads=[xb], writes=xT_bufs(c0, n), dma=True)

    sa_p = Pool_(2, [128, BLK], F32, "sa")
    def ffn_part(l, c0, n, wg, wu, wd, f0, f1, gmask):
        nf = f1 - f0
        for fc in range(f0, f1):
            tg, bg = load_w(wg, fc)
            tu, bu = load_w(wu, fc)
            def mm(e, tg=tg, tu=tu):
                for kc in range(KC):
                    e.matmul(pbank[0][:, :n], lhsT=tg[:, kc, :], rhs=hT[:, kc, :n], start=(kc == 0), stop=(kc == KC - 1))
                for kc in range(KC):
                    ins = e.matmul(pbank[1][:, :n], lhsT=tu[:, kc, :], rhs=hT[:, kc, :n], start=(kc == 0), stop=(kc == KC - 1))
                return ins
            pa, pu = (0, 1) if (fc - f0) % 2 == 0 else (2, 3)
            def mm2(e, tg=tg, tu=tu, pa=pa, pu=pu):
                for kc in range(KC):
                    e.matmul(pbank[pa][:, :n], lhsT=tg[:, kc, :], rhs=hT[:, kc, :n], start=(kc == 0), stop=(kc == KC - 1))
                for kc in range(KC):
                    ins = e.matmul(pbank[pu][:, :n], lhsT=tu[:, kc, :], rhs=hT[:, kc, :n], start=(kc == 0), stop=(kc == KC - 1))
                return ins
            P.op("pe", mm2, reads=[bg, bu, hT_b], writes=[pbuf[pa], pbuf[pu]])
            sa, sab = sa_p.get()
            P.op("act", lambda e, sa=sa, pa=pa: e.activation(out=sa[:, :n], in_=pbank[pa][:, :n], func=AF.Silu), reads=[pbuf[pa]], writes=[sab])
            P.op("dve", lambda e, sa=sa, pu=pu, fc=fc: e.tensor_tensor(out=hb[:, fc - f0, :n], in0=pbank[pu][:, :n], in1=sa[:, :n], op=ALU.mult),
                 reads=[pbuf[pu], sab], writes=[hb_b])
        pre = resid_load(0, c0, n)
        for dc in range(KC):
            t, b = wdpool.get()
            P.op("sp", lambda e, t=t, dc=dc: e.dma_start(out=t[:, :nf, :], in_=wd.dst[dc][:, f0:f1, :]), reads=[wd.bufs[dc]], writes=[b], dma=True)
            pb = 6 + dc % 2
            def mm(e, t=t, pb=pb):
                for k in range(nf):
                    ins = e.matmul(pbank[pb][:, :n], lhsT=t[:, k, :], rhs=hb[:, k, :n], start=(k == 0), stop=(k == nf - 1))
                return ins
            P.op("pe", mm, reads=[b, hb_b], writes=[pbuf[pb]])
            nxt = resid_load(dc + 1, c0, n) if dc + 1 < KC else None
            resid_fin(l, 5, dc, c0, n, pb, gmask, pre)
            pre = nxt

    def split(n, parts):
        q, r = divmod(n, parts)
        res, o = [], 0
        for i in range(parts):
            s = q + (1 if i < r else 0)
            if s:
                res.append((o, o + s))
            o += s
        return res

    lg = sb([128, NSUB, NE], F32, "lg"); lg2 = sb([128, NSUB, NE], F32, "lg2")
    mk1 = sb([128, NSUB, NE], F32, "mk1"); mk2 = sb([128, NSUB, NE], F32, "mk2"); G = sb([128, NSUB, NE], F32, "G")
    m1 = sb([128, NSUB], F32, "m1"); m2_ = sb([128, NSUB], F32, "m2"); g1 = sb([128, NSUB], F32, "g1"); g2 = sb([128, NSUB], F32, "g2")
    GT = sb([NE, BLK], F32, "GT")
    rt_b = Buf("router")

    def ffn(l, c0, n):
        moe = (l % 2 == 1)
        norm_mod(l, 1, c0, n, router=moe)
        if not moe:
            for (f0, f1) in split(FC, 4):
                ffn_part(l, c0, n, W_fg, W_fu, W_fd, f0, f1, None)
            return
        for s in range(NSUB):
            P.op("dve", lambda e, s=s: e.tensor_copy(out=lg[:, s, :], in_=pbank[s][:, 0:NE]), reads=[pbuf[s]], writes=[rt_b])
        P.op("dve", lambda e: e.tensor_reduce(out=m1[:], in_=lg[:], axis=AX.X, op=ALU.max), reads=[rt_b], writes=[rt_b])
        for s in range(NSUB):
            P.op("dve", lambda e, s=s: e.tensor_scalar(out=mk1[:, s, :], in0=lg[:, s, :], scalar1=m1[:, s:s + 1], scalar2=None, op0=ALU.is_equal), reads=[rt_b], writes=[rt_b])
        P.op("dve", lambda e: e.scalar_tensor_tensor(out=lg2[:], in0=mk1[:], scalar=-1e30, in1=lg[:], op0=ALU.mult, op1=ALU.add), reads=[rt_b], writes=[rt_b])
        P.op("dve", lambda e: e.tensor_reduce(out=m2_[:], in_=lg2[:], axis=AX.X, op=ALU.max), reads=[rt_b], writes=[rt_b])
        for s in range(NSUB):
            P.op("dve", lambda e, s=s: e.tensor_scalar(out=mk2[:, s, :], in0=lg2[:, s, :], scalar1=m2_[:, s:s + 1], scalar2=None, op0=ALU.is_equal), reads=[rt_b], writes=[rt_b])
        P.op("dve", lambda e: e.tensor_tensor(out=g2[:], in0=m2_[:], in1=m1[:], op=ALU.subtract), reads=[rt_b], writes=[rt_b])
        P.op("act", lambda e: e.activation(out=g2[:], in_=g2[:], func=AF.Exp), reads=[rt_b], writes=[rt_b])
        P.op("dve", lambda e: e.tensor_scalar(out=g1[:], in0=g2[:], scalar1=1.0, scalar2=None, op0=ALU.add), reads=[rt_b], writes=[rt_b])
        P.op("dve", lambda e: e.reciprocal(out=g1[:], in_=g1[:]), reads=[rt_b], writes=[rt_b])
        P.op("dve", lambda e: e.tensor_tensor(out=g2[:], in0=g2[:], in1=g1[:], op=ALU.mult), reads=[rt_b], writes=[rt_b])
        for s in range(NSUB):
            P.op("dve", lambda e, s=s: e.tensor_scalar(out=G[:, s, :], in0=mk1[:, s, :], scalar1=g1[:, s:s + 1], scalar2=None, op0=ALU.mult), reads=[rt_b], writes=[rt_b])
            P.op("dve", lambda e, s=s: e.scalar_tensor_tensor(out=G[:, s, :], in0=mk2[:, s, :], scalar=g2[:, s:s + 1], in1=G[:, s, :], op0=ALU.mult, op1=ALU.add), reads=[rt_b], writes=[rt_b])
        for s in range(NSUB):
            P.op("pe", lambda e, s=s: e.transpose(pbank[4][0:NE, s * 128:(s + 1) * 128], G[:, s, :], ident[:]), reads=[rt_b, cb], writes=[pbuf[4]])
        P.op("act", lambda e: e.activation(out=GT[:], in_=pbank[4][0:NE, :n], func=AF.Copy), reads=[pbuf[4]], writes=[rt_b])
        for ex in range(NE):
            pb = 4 + ex % 2
            P.op("pe", lambda e, ex=ex, pb=pb: e.matmul(pbank[pb][:, :n], lhsT=sel[:, ex * 128:(ex + 1) * 128], rhs=GT[:, :n], start=True, stop=True), reads=[rt_b, cb], writes=[pbuf[pb]])
            P.op("act", lambda e, ex=ex, pb=pb: e.activation(out=Gbc[:, 0, :], in_=pbank[pb][:, :n], func=AF.Copy), reads=[pbuf[pb]], writes=[Gbc_b])
            for (f0, f1) in split(FE, 2):
                ffn_part(l, c0, n, W_mg[ex], W_mu[ex], W_md[ex], f0, f1, Gbc[:, 0, :])

    def final(tt):
        c0, n = tt * 128, 128
        rms_stats(c0, n, [4])
        xn = SCR[:, 0:D].rearrange("p (k c) -> p k c", c=128)
        ot = SCR[:, D:2 * D]
        for kc in range(KC):
            xc, xb = xcp.get()
            P.op("act", lambda e, xc=xc, kc=kc: e.dma_start(out=xc[:, :n], in_=xT[kc, :, c0:c0 + n]), reads=xT_bufs(c0, n), writes=[xb], dma=True)
            P.op("dve", lambda e, xc=xc, kc=kc: e.scalar_tensor_tensor(out=xn[:, kc, :], in0=xc[:, :n], scalar=V("fg", kc), in1=rstd[:, :n], op0=ALU.mult, op1=ALU.mult),
                 reads=[xb, rstd_b, cb], writes=[scr_b[0]])
        for c4 in range(KC // 4):
            pb = c4 % 4
            def tr(e, c4=c4, pb=pb):
                for q in range(4):
                    ins = e.transpose(pbank[pb][:, q * 128:(q + 1) * 128], xn[:, c4 * 4 + q, :], ident[:])
                return ins
            P.op("pe", tr, reads=[scr_b[0], cb], writes=[pbuf[pb]])
            if c4 % 2 == 0:
                P.op("act", lambda e, c4=c4, pb=pb: e.activation(out=ot[:, c4 * 512:(c4 + 1) * 512], in_=pbank[pb][:], func=AF.Copy), reads=[pbuf[pb]], writes=[scr_b[1]])
            else:
                P.op("dve", lambda e, c4=c4, pb=pb: e.tensor_copy(out=ot[:, c4 * 512:(c4 + 1) * 512], in_=pbank[pb][:]), reads=[pbuf[pb]], writes=[scr_b[1]])
        return [P.op("act", lambda e: e.dma_start(out=out_d[c0 - HALO:c0 - HALO + 128, :], in_=ot), reads=[scr_b[1]], dma=True)]

    main = [(HALO + i * BLK, BLK) for i in range(NB)]
    for l in range(L):
        for i in reversed(range(NB)):
            c0, n = main[i]
            mixer(l, c0, n, HALO, mask=(i == 0))
        if l + 1 < L:
            mixer(l, 0, HALO, 0, mask=False)
            ffn(l, 0, HALO)
        for (c0, n) in main:
            ffn(l, c0, n)
    alltoks = []
    for tt in range(1, T // 128):
        alltoks += final(tt)
    P.final_wait("act", alltoks[-12:])
    P.emit_all()
    P.close()
    es.close()
    return nc


def _pack_inputs(cfg, b, inp, hm):
    KC, GC, NE, L = cfg.KC, cfg.GC, cfg.NE, cfg.L
    vecs = np.zeros((128, cfg.NV), np.float32)
    def put(name, arr):
        o, n = cfg.off[name]
        a = np.asarray(arr, np.float32)
        if a.ndim == 1:
            vecs[:, o:o + n] = a.reshape(-1, 128).T
        else:
            k = a.shape[0]
            vecs[:, o:o + n] = a.reshape(k, -1, 128).transpose(2, 0, 1).reshape(128, -1)
    for l in range(L):
        put(f"gmix{l}", inp["norm_mix_g"][l]); put(f"gffn{l}", inp["norm_ffn_g"][l]); put(f"bada{l}", inp["b_ada"][l])
        put(f"scw{l}", inp["sc_conv_w"][l]); put(f"cfw{l}", inp["cf_conv_w"][l])
        put(f"cfb{l}", inp["cf_conv_b"][l]); put(f"cfg{l}", inp["cf_ln_g"][l]); put(f"cfbeta{l}", inp["cf_ln_b"][l])
    put("fg", inp["final_g"]); put("c", inp["c"][b])
    o, n = cfg.off["hmask"]
    vecs[:, o] = hm
    o, n = cfg.off["rw"]
    rw = np.asarray(inp["router_w"][0], np.float32)
    vecs[:, o:o + n] = rw.reshape(KC, 128, NE).transpose(1, 0, 2).reshape(128, KC * NE)
    return vecs


_CACHE = {}


def run(cfg, inputs):
    key = (cfg.D, cfg.DFF, cfg.DFE, cfg.S, cfg.BLK, cfg.NCPB)
    if key not in _CACHE:
        _CACHE[key] = build(cfg)
    nc = _CACHE[key]
    NE = cfg.NE
    sel = np.zeros((NE, NE * 128), np.float32)
    for e in range(NE):
        sel[e, e * 128:(e + 1) * 128] = 1.0
    B = inputs["x"].shape[0]
    in_maps = []
    f = lambda a: np.ascontiguousarray(np.asarray(a, np.float32))
    shared = dict(
        ident=np.eye(128, dtype=np.float32), ones=np.ones((128, 128), np.float32), sel=sel,
        w_ada=f(inputs["w_ada"]), w_in=f(inputs["w_in"]), w_out=f(inputs["w_out"]),
        ffn_w_gate=f(inputs["ffn_w_gate"][0]), ffn_w_up=f(inputs["ffn_w_up"][0]), ffn_w_down=f(inputs["ffn_w_down"][0]),
        moe_w_gate=f(inputs["moe_w_gate"][0]), moe_w_up=f(inputs["moe_w_up"][0]), moe_w_down=f(inputs["moe_w_down"][0]),
    )
    NCPB, TM, T = cfg.NCPB, cfg.TM, cfg.T
    x = np.asarray(inputs["x"], np.float32)
    for b in range(B):
        for j in range(NCPB):
            m = dict(shared)
            xs = np.zeros((T, cfg.D), np.float32)
            lo = j * TM
            if j > 0:
                xs[:] = x[b, lo - 128:lo + TM]
            else:
                xs[128:] = x[b, 0:TM]
            m["x"] = xs
            m["vecs"] = _pack_inputs(cfg, b, inputs, 0.0 if j == 0 else 1.0)
            in_maps.append(m)
    ncores = B * NCPB
    res = run_bass_kernel_spmd(nc, in_maps, core_ids=list(range(ncores)))
    out = np.empty((B, cfg.S, cfg.D), np.float32)
    for b in range(B):
        for j in range(NCPB):
            out[b, j * TM:(j + 1) * TM] = res.results[b * NCPB + j]["out"]
    return out


def kernel(**inputs):
    cfg = Cfg()
    return run(cfg, inputs)
```

```python
import numpy as np
from contextlib import ExitStack
import concourse.bass as bass
import concourse.mybir as mybir
from concourse.bass_utils import run_bass_kernel_spmd

F32 = mybir.dt.float32
BF16 = mybir.dt.bfloat16
AF = mybir.ActivationFunctionType
ALU = mybir.AluOpType
AX = mybir.AxisListType

SEM_EPOCH = 30000


class Buf:
    __slots__ = ("name", "last_write", "reads")

    def __init__(self, name=""):
        self.name = name
        self.last_write = None
        self.reads = []


class Prog:
    ENG = ("pe", "act", "dve", "pool", "sp")

    def __init__(self, nc, ndma_sems=6):
        self.nc = nc
        self.ops = {e: [] for e in self.ENG}
        self.sems = []
        self._sem_ctx = []
        self.cur = {}
        self.waited = {e: {} for e in self.ENG}
        self.dma_pool = {}
        self.dma_rr = {e: 0 for e in self.ENG}
        self.ndma = ndma_sems

    def _new_sem(self, name):
        cm = self.nc.semaphore(name)
        h = cm.__enter__()
        self._sem_ctx.append(cm)
        self.sems.append(h)
        return len(self.sems) - 1

    def _compute_token(self, eng):
        st = self.cur.get(eng)
        if st is None or st[1] >= SEM_EPOCH:
            st = [self._new_sem(f"s_{eng}_{len(self.sems)}"), 0]
            self.cur[eng] = st
        st[1] += 1
        return (st[0], st[1])

    def _dma_token(self, q):
        pool = self.dma_pool.setdefault(q, [])
        if len(pool) < self.ndma:
            pool.append([self._new_sem(f"d_{q}_{len(pool)}"), 0, None])
        i = self.dma_rr[q] % self.ndma
        self.dma_rr[q] += 1
        ent = pool[i]
        prev = ent[2]
        if ent[1] >= SEM_EPOCH * 16:
            ent[0] = self._new_sem(f"d_{q}_{i}_{len(self.sems)}")
            ent[1] = 0
        ent[1] += 16
        tok = (ent[0], ent[1])
        ent[2] = tok
        return tok, prev

    def op(self, eng, emit, reads=(), writes=(), dma=False):
        deps = set()
        for b in reads:
            if b.last_write is not None:
                deps.add(b.last_write)
        for b in writes:
            if b.last_write is not None:
                deps.add(b.last_write)
            for t in b.reads:
                deps.add(t)
        if dma:
            tok, prev = self._dma_token(eng)
            if prev is not None:
                deps.add(prev)
            inc = 16
        else:
            tok = self._compute_token(eng)
            inc = 1
        own = self.cur[eng][0] if (not dma and eng in self.cur) else None
        w = self.waited[eng]
        need = {}
        for (s, v) in deps:
            if eng == "pe" and not dma and s == own:
                continue
            if need.get(s, 0) < v:
                need[s] = v
        waits = []
        for s, v in need.items():
            if w.get(s, 0) >= v:
                continue
            w[s] = v
            waits.append((s, v))
        self.ops[eng].append((waits, emit, tok, inc))
        for b in reads:
            b.reads.append(tok)
        for b in writes:
            b.last_write = tok
            b.reads = []
        return tok

    def final_wait(self, eng, toks):
        self.ops[eng].append((list(toks), None, None, 0))

    def emit_all(self):
        nc, sems, ops = self.nc, self.sems, self.ops
        with nc.Block() as block:
            def run(engname):
                def body(e):
                    for (waits, emit, tok, inc) in ops[engname]:
                        for (s, v) in waits:
                            e.wait_ge(sems[s], v)
                        if emit is not None:
                            emit(e).then_inc(sems[tok[0]], inc)
                return body
            block.sync(run("sp"))
            block.scalar(run("act"))
            block.vector(run("dve"))
            block.gpsimd(run("pool"))
            block.tensor(run("pe"))

    def close(self):
        for cm in reversed(self._sem_ctx):
            cm.__exit__(None, None, None)


class Cfg:
    def __init__(self, D=4096, DFF=11008, DFE=4096, NE=8, S=8192, BLK=512, L=2, NCPB=4):
        self.D, self.DFF, self.DFE, self.NE, self.S, self.BLK, self.L = D, DFF, DFE, NE, S, BLK, L
        self.NCPB = NCPB
        self.TM = S // NCPB
        self.T = self.TM + 128
        self.KC = D // 128
        self.GC = D // 256
        self.FC = DFF // 128
        self.FE = DFE // 128
        self.NB = self.TM // BLK
        KC, GC = self.KC, self.GC
        off = {}
        c = 0
        def add(name, n):
            nonlocal c
            off[name] = (c, n)
            c += n
        for l in range(L):
            add(f"gmix{l}", KC); add(f"gffn{l}", KC); add(f"bada{l}", 6 * KC)
            add(f"scw{l}", 3 * GC); add(f"cfw{l}", 31 * GC)
            add(f"cfb{l}", GC); add(f"cfg{l}", GC); add(f"cfbeta{l}", GC)
        add("fg", KC); add("c", KC); add("rw", KC * NE); add("hmask", 1)
        self.off = off
        self.NV = c


def build(cfg):
    D, KC, GC, FC, FE, NE, S, BLK, L, NB = cfg.D, cfg.KC, cfg.GC, cfg.FC, cfg.FE, cfg.NE, cfg.S, cfg.BLK, cfg.L, cfg.NB
    DFF, DFE = cfg.DFF, cfg.DFE
    HALO = 128
    T, TM = cfg.T, cfg.TM
    NSUB = BLK // 128
    nc = bass.Bass("TRN2", target_bir_lowering=False)
    def din(name, shape):
        return nc.dram_tensor(name, shape, F32, kind="ExternalInput").ap()
    x_in = din("x", [T, D])
    vecs_d = din("vecs", [128, cfg.NV])
    ident_d = din("ident", [128, 128])
    ones_d = din("ones", [128, 128])
    sel_d = din("sel", [NE, NE * 128])
    w_ada = din("w_ada", [L, D, 6 * D])
    w_in = din("w_in", [L, D, 5 * D // 2])
    w_out = din("w_out", [L, D, D])
    f_gate = din("ffn_w_gate", [D, DFF])
    f_up = din("ffn_w_up", [D, DFF])
    f_down = din("ffn_w_down", [DFF, D])
    m_gate = din("moe_w_gate", [NE, D, DFE])
    m_up = din("moe_w_up", [NE, D, DFE])
    m_down = din("moe_w_down", [NE, DFE, D])
    out_d = nc.dram_tensor("out", [TM, D], F32, kind="ExternalOutput").ap()
    xT = nc.dram_tensor("xT_scr", [KC, 128, T], F32).ap()

    es = ExitStack()
    P = Prog(nc)
    cnt = [0]
    def sb(shape, dt, name=None):
        cnt[0] += 1
        return es.enter_context(nc.sbuf_tensor(name or f"t{cnt[0]}", shape, dt))
    class Pool_:
        def __init__(self, n, shape, dt, name):
            self.t = [sb(shape, dt, f"{name}{i}") for i in range(n)]
            self.b = [Buf(f"{name}{i}") for i in range(n)]
            self.i = 0
        def get(self):
            k = self.i % len(self.t)
            self.i += 1
            return self.t[k], self.b[k]
    NWMAX = BLK + HALO
    ident = sb([128, 128], F32, "ident_s"); ones = sb([128, 128], F32, "ones_s")
    vecs = sb([128, cfg.NV], F32, "vecs_s"); sel = sb([NE, NE * 128], F32, "sel_s")
    epsr = sb([128, 1], F32, "epsr"); epsl = sb([128, 1], F32, "epsl")
    cb = Buf("const")
    P.op("sp", lambda e: e.dma_start(out=ident[:], in_=ident_d), writes=[cb], dma=True)
    P.op("sp", lambda e: e.dma_start(out=ones[:], in_=ones_d), writes=[cb], dma=True)
    P.op("sp", lambda e: e.dma_start(out=vecs[:], in_=vecs_d), writes=[cb], dma=True)
    P.op("sp", lambda e: e.dma_start(out=sel[:], in_=sel_d), writes=[cb], dma=True)
    P.op("dve", lambda e: e.memset(epsr[:], 1e-6), writes=[cb])
    P.op("dve", lambda e: e.memset(epsl[:], 1e-5), writes=[cb])
    def V(name, j=None, n=1):
        o, ln = cfg.off[name]
        if j is None:
            return vecs[:, o:o + ln]
        return vecs[:, o + j:o + j + n]
    pbank = [es.enter_context(nc.psum_tensor(f"ps{i}", [128, 512], F32)) for i in range(8)]
    pbuf = [Buf(f"ps{i}") for i in range(8)]
    wpool = Pool_(4, [128, KC, 128], BF16, "w")
    xcp = Pool_(3, [128, NWMAX], F32, "xc")
    tmpp = Pool_(4, [128, NWMAX], F32, "tmp")
    hT = sb([128, KC, NWMAX], BF16, "hT"); hT_b = Buf("hT")
    yT = sb([128, KC, BLK], BF16, "yT"); yT_b = Buf("yT")
    SCR = sb([128, 2 * D], F32, "SCR"); scr_b = [Buf("scr0"), Buf("scr1")]
    assert GC * BLK <= 2 * D
    u2T = SCR[:, 0:GC * BLK].rearrange("p (g t) -> p g t", g=GC)
    u2_b = [scr_b[(g * BLK) // D] for g in range(GC)]
    class ScrPool:
        def __init__(self, view):
            self.i = 0; self.view = view
        def get(self):
            k = self.i % 2; self.i += 1
            return self.view(SCR[:, k * D:(k + 1) * D]), scr_b[k]
    wapool = ScrPool(lambda a: a.rearrange("p (k c) -> p k c", c=512))
    xin_p = ScrPool(lambda a: a)
    NHB = max((FC + 2) // 3, FE)
    assert NHB <= KC
    hb = yT; hb_b = yT_b
    rstd = sb([128, NWMAX], F32, "rstd"); rstd_b = Buf("rstd")
    mu = sb([128, BLK], F32, "mu"); mu_b = Buf("mu")
    Gbc = sb([128, 1, BLK], F32, "Gbc"); Gbc_b = Buf("Gbc")
    mods = [sb([128, 6 * KC], F32, f"mod{l}") for l in range(L)]; mod_b = [Buf() for _ in range(L)]
    gs = [[sb([128, KC], F32, f"gs{l}_{i}") for i in range(2)] for l in range(L)]
    cact = sb([128, KC], F32, "cact")

    class WT:
        def __init__(self, name, src, K, N):
            self.src, self.kch, self.nt = src, K // 128, N // 128
            self.dst = nc.dram_tensor("wb_" + name, [self.nt, 128, self.kch, 128], BF16).ap()
            self.bufs = [Buf(f"{name}{j}") for j in range(self.nt)]
        def prepass(self, js=None):
            for j in (range(self.nt) if js is None else js):
                P.op("pool", lambda e, j=j: e.dma_start(out=self.dst[j], in_=wtile_view(self.src, j)), writes=[self.bufs[j]], dma=True)
    W_in = [WT(f"in{l}", w_in[l], D, 5 * D // 2) for l in range(L)]
    W_out = [WT(f"out{l}", w_out[l], D, D) for l in range(L)]
    W_fg, W_fu, W_fd = WT("fg", f_gate, D, DFF), WT("fu", f_up, D, DFF), WT("fd", f_down, DFF, D)
    W_mg = [WT(f"mg{e}", m_gate[e], D, DFE) for e in range(NE)]
    W_mu = [WT(f"mu{e}", m_up[e], D, DFE) for e in range(NE)]
    W_md = [WT(f"md{e}", m_down[e], DFE, D) for e in range(NE)]

    def wtile_view(wap, j):
        return wap.rearrange("(kc p) n -> p kc n", p=128)[:, :, j * 128:(j + 1) * 128]

    def load_w(wt, j):
        t, b = wpool.get()
        P.op("sp", lambda e: e.dma_start(out=t[:], in_=wt.dst[j]), reads=[wt.bufs[j]], writes=[b], dma=True)
        return t, b

    def ntiles(n):
        r = []
        o = 0
        while o < n:
            r.append((o, min(512, n - o)))
            o += 512
        return r

    P.op("act", lambda e: e.activation(out=cact[:], in_=V("c"), func=AF.Silu), reads=[cb], writes=[cb])
    KQ = KC // 4
    for l in range(L):
        wav = w_ada[l].rearrange("(kc p) n -> p kc n", p=128)
        for grp in range(6 * KC // 4):
            for st in range(4):
                t, b = wapool.get()
                P.op("sp", lambda e, t=t, st=st, grp=grp, wav=wav: e.dma_start(out=t[:, 0:KQ, :], in_=wav[:, st * KQ:(st + 1) * KQ, grp * 512:(grp + 1) * 512]), writes=[b], dma=True)
                def mm(e, t=t, st=st):
                    for c in range(4):
                        for kq in range(KQ):
                            ins = e.matmul(pbank[c][:, 0:1], lhsT=t[:, kq, c * 128:(c + 1) * 128], rhs=cact[:, st * KQ + kq:st * KQ + kq + 1],
                                           start=(st == 0 and kq == 0), stop=(st == 3 and kq == KQ - 1))
                    return ins
                P.op("pe", mm, reads=[b, cb], writes=[pbuf[c] for c in range(4)])
            for c in range(4):
                j = grp * 4 + c
                P.op("dve", lambda e, l=l, j=j, c=c: e.tensor_tensor(out=mods[l][:, j:j + 1], in0=pbank[c][:, 0:1], in1=V(f"bada{l}", j), op=ALU.add),
                     reads=[pbuf[c], cb], writes=[mod_b[l]])
        P.op("dve", lambda e, l=l: e.scalar_tensor_tensor(out=gs[l][0][:], in0=mods[l][:, KC:2 * KC], scalar=1.0, in1=V(f"gmix{l}"), op0=ALU.add, op1=ALU.mult),
             reads=[mod_b[l], cb], writes=[mod_b[l]])
        P.op("dve", lambda e, l=l: e.scalar_tensor_tensor(out=gs[l][1][:], in0=mods[l][:, 4 * KC:5 * KC], scalar=1.0, in1=V(f"gffn{l}"), op0=ALU.add, op1=ALU.mult),
             reads=[mod_b[l], cb], writes=[mod_b[l]])

    for l in range(L):
        W_in[l].prepass(); W_out[l].prepass()
        if l % 2 == 0:
            for fc in range(FC):
                W_fg.prepass([fc]); W_fu.prepass([fc])
            W_fd.prepass()
        else:
            for ex in range(NE):
                for fc in range(FE):
                    W_mg[ex].prepass([fc]); W_mu[ex].prepass([fc])
                W_md[ex].prepass()
    xT_b = [Buf(f"xT{b}") for b in range(T // 128)]

    xts_p = Pool_(2, [128, 4, 128], F32, "xts")
    for tt in range(T // 128):
        blk = tt
        t, b = xin_p.get()
        P.op("act", lambda e, t=t, tt=tt: e.dma_start(out=t[:], in_=x_in[tt * 128:(tt + 1) * 128, :]), writes=[b], dma=True)
        for c4 in range(KC // 4):
            pb = c4 % 4
            def tr(e, t=t, c4=c4, pb=pb):
                for q in range(4):
                    ins = e.transpose(pbank[pb][:, q * 128:(q + 1) * 128], t[:, (c4 * 4 + q) * 128:(c4 * 4 + q + 1) * 128], ident[:])
                return ins
            P.op("pe", tr, reads=[b, cb], writes=[pbuf[pb]])
            ts_, tb_ = xts_p.get()
            eng = "act" if c4 % 2 == 0 else "dve"
            if eng == "act":
                P.op("act", lambda e, ts_=ts_, pb=pb: e.activation(out=ts_[:].rearrange("p a b -> p (a b)"), in_=pbank[pb][:], func=AF.Copy), reads=[pbuf[pb]], writes=[tb_])
            else:
                P.op("dve", lambda e, ts_=ts_, pb=pb: e.tensor_copy(out=ts_[:].rearrange("p a b -> p (a b)"), in_=pbank[pb][:]), reads=[pbuf[pb]], writes=[tb_])
            P.op("act", lambda e, ts_=ts_, c4=c4, tt=tt: e.dma_start(out=xT[c4 * 4:(c4 + 1) * 4].rearrange("c p t -> p c t")[:, :, tt * 128:(tt + 1) * 128], in_=ts_[:]),
                 reads=[tb_], writes=[xT_b[blk]], dma=True)

    def xT_bufs(c0, n):
        return [xT_b[b] for b in range(c0 // 128, (c0 + n - 1) // 128 + 1)]

    def rms_stats(c0, n, pbs):
        tl = ntiles(n)
        for kc in range(KC):
            xc, xb = xcp.get()
            P.op("act", lambda e, xc=xc, kc=kc: e.dma_start(out=xc[:, :n], in_=xT[kc, :, c0:c0 + n]), reads=xT_bufs(c0, n), writes=[xb], dma=True)
            sq, sqb = tmpp.get()
            P.op("act", lambda e, xc=xc, sq=sq: e.activation(out=sq[:, :n], in_=xc[:, :n], func=AF.Square), reads=[xb], writes=[sqb])
            def mm(e, sq=sq, kc=kc):
                for i, (o, w) in enumerate(tl):
                    ins = e.matmul(pbank[pbs[i]][:, :w], lhsT=ones[:], rhs=sq[:, o:o + w], start=(kc == 0), stop=(kc == KC - 1))
                return ins
            P.op("pe", mm, reads=[sqb, cb], writes=[pbuf[pbs[i]] for i in range(len(tl))])
        for i, (o, w) in enumerate(tl):
            P.op("act", lambda e, i=i, o=o, w=w: e.activation(out=rstd[:, o:o + w], in_=pbank[pbs[i]][:, :w], func=AF.Sqrt, scale=1.0 / D, bias=epsr[:]),
                 reads=[pbuf[pbs[i]], cb], writes=[rstd_b])
        P.op("dve", lambda e: e.reciprocal(out=rstd[:, :n], in_=rstd[:, :n]), reads=[rstd_b], writes=[rstd_b])

    def norm_mod(l, which, c0, n, router=False):
        rms_stats(c0, n, [4, 5])
        shift = mods[l][:, (0 if which == 0 else 3 * KC):]
        gsv = gs[l][which]
        for kc in range(KC):
            xc, xb = xcp.get()
            P.op("act", lambda e, xc=xc, kc=kc: e.dma_start(out=xc[:, :n], in_=xT[kc, :, c0:c0 + n]), reads=xT_bufs(c0, n), writes=[xb], dma=True)
            tm, tmb = tmpp.get()
            P.op("dve", lambda e, xc=xc, tm=tm: e.tensor_tensor(out=tm[:, :n], in0=xc[:, :n], in1=rstd[:, :n], op=ALU.mult), reads=[xb, rstd_b], writes=[tmb])
            if not router:
                P.op("act", lambda e, tm=tm, kc=kc: e.activation(out=hT[:, kc, :n], in_=tm[:, :n], func=AF.Identity, scale=gsv[:, kc:kc + 1], bias=shift[:, kc:kc + 1]),
                     reads=[tmb, mod_b[l]], writes=[hT_b])
            else:
                hf, hfb = tmpp.get()
                P.op("act", lambda e, tm=tm, kc=kc, hf=hf: e.activation(out=hf[:, :n], in_=tm[:, :n], func=AF.Identity, scale=gsv[:, kc:kc + 1], bias=shift[:, kc:kc + 1]),
                     reads=[tmb, mod_b[l]], writes=[hfb])
                P.op("dve", lambda e, kc=kc, hf=hf: e.tensor_copy(out=hT[:, kc, :n], in_=hf[:, :n]), reads=[hfb], writes=[hT_b])
                def mm(e, hf=hf, kc=kc):
                    for s in range(n // 128):
                        ins = e.matmul(pbank[s][:, 0:NE], lhsT=hf[:, s * 128:(s + 1) * 128], rhs=V("rw")[:, kc * NE:(kc + 1) * NE], start=(kc == 0), stop=(kc == KC - 1))
                    return ins
                P.op("pe", mm, reads=[hfb, cb], writes=[pbuf[s] for s in range(n // 128)])

    zc_p = Pool_(2, [128, NWMAX], F32, "zc")
    acc_p = Pool_(2, [128, BLK], F32, "acc")
    def inproj(l, j, nw, pbs, lo=0):
        t, b = load_w(W_in[l], j)
        tl = ntiles(nw)
        def mm(e, t=t):
            for i, (o, w) in enumerate(tl):
                for kc in range(KC):
                    ins = e.matmul(pbank[pbs[i]][:, :w], lhsT=t[:, kc, :], rhs=hT[:, kc, lo + o:lo + o + w], start=(kc == 0), stop=(kc == KC - 1))
            return ins
        P.op("pe", mm, reads=[b, hT_b], writes=[pbuf[pbs[i]] for i in range(len(tl))])
        return tl

    def conv(src, H, n_out, wname, l, g, K, acc):
        wv = V(wname + str(l))
        P_ = []
        for k in range(K - 1, -1, -1):
            sh = K - 1 - k
            o0 = max(0, sh - H)
            if o0 >= n_out:
                continue
            wcol = wv[:, k * GC + g:k * GC + g + 1]
            if k == K - 1:
                P_.append(lambda e, wcol=wcol: e.tensor_scalar(out=acc[:, :n_out], in0=src[:, H:H + n_out], scalar1=wcol, scalar2=None, op0=ALU.mult))
            else:
                P_.append(lambda e, wcol=wcol, o0=o0, sh=sh: e.scalar_tensor_tensor(out=acc[:, o0:n_out], in0=src[:, H + o0 - sh:H + n_out - sh], scalar=wcol,
                                                                               in1=acc[:, o0:n_out], op0=ALU.mult, op1=ALU.add))
        return P_

    def mixer(l, c0, n_out, H, mask):
        nw = n_out + H
        norm_mod(l, 0, c0 - H, nw)
        pend_stats = []
        for g in range(GC):
            tl = inproj(l, GC + g, nw, [0, 1])
            zc, zcb = zc_p.get()
            for i, (o, w) in enumerate(tl):
                P.op("act", lambda e, i=i, o=o, w=w, zc=zc: e.activation(out=zc[:, o:o + w], in_=pbank[i][:, :w], func=AF.Copy), reads=[pbuf[i]], writes=[zcb])
            tl = inproj(l, 2 * GC + g, nw, [2, 3])
            cv, cvb = tmpp.get()
            for i, (o, w) in enumerate(tl):
                P.op("dve", lambda e, i=i, o=o, w=w, zc=zc, cv=cv: e.tensor_tensor(out=cv[:, o:o + w], in0=pbank[2 + i][:, :w], in1=zc[:, o:o + w], op=ALU.mult),
                     reads=[pbuf[2 + i], zcb], writes=[cvb])
            if mask:
                P.op("dve", lambda e, cv=cv: e.tensor_scalar(out=cv[:, 0:H], in0=cv[:, 0:H], scalar1=V("hmask"), scalar2=None, op0=ALU.mult), reads=[cvb, cb], writes=[cvb])
            acc, accb = acc_p.get()
            for f in conv(cv, H, n_out, "scw", l, g, 3, acc):
                P.op("dve", f, reads=[cvb, cb], writes=[accb])
            tl = inproj(l, g, n_out, [0], lo=H)
            P.op("dve", lambda e, acc=acc, g=g: e.tensor_tensor(out=yT[:, g, :n_out], in0=pbank[0][:, :n_out], in1=acc[:, :n_out], op=ALU.mult),
                 reads=[pbuf[0], accb], writes=[yT_b])
            tl = inproj(l, 4 * GC + g, nw, [2, 3])
            sg, sgb = zc_p.get()
            for i, (o, w) in enumerate(tl):
                P.op("act", lambda e, i=i, o=o, w=w, sg=sg: e.activation(out=sg[:, o:o + w], in_=pbank[2 + i][:, :w], func=AF.Sigmoid), reads=[pbuf[2 + i]], writes=[sgb])
            tl = inproj(l, 3 * GC + g, nw, [0, 1])
            u, ub = tmpp.get()
            for i, (o, w) in enumerate(tl):
                P.op("dve", lambda e, i=i, o=o, w=w, sg=sg, u=u: e.tensor_tensor(out=u[:, o:o + w], in0=pbank[i][:, :w], in1=sg[:, o:o + w], op=ALU.mult),
                     reads=[pbuf[i], sgb], writes=[ub])
            if mask:
                P.op("dve", lambda e, u=u: e.tensor_scalar(out=u[:, 0:H], in0=u[:, 0:H], scalar1=V("hmask"), scalar2=None, op0=ALU.mult), reads=[ub, cb], writes=[ub])
            acc, accb = acc_p.get()
            for f in conv(u, H, n_out, "cfw", l, g, 31, acc):
                P.op("dve", f, reads=[ub, cb], writes=[accb])
            P.op("act", lambda e, acc=acc, g=g: e.activation(out=u2T[:, g, :n_out], in_=acc[:, :n_out], func=AF.Identity, bias=V(f"cfb{l}", g), scale=1.0),
                 reads=[accb, cb], writes=[u2_b[g]])
            sq, sqb = tmpp.get()
            P.op("act", lambda e, sq=sq, g=g: e.activation(out=sq[:, :n_out], in_=u2T[:, g, :n_out], func=AF.Square), reads=[u2_b[g]], writes=[sqb])
            def mmst(e, g=g, sq=sq):
                e.matmul(pbank[4][:, :n_out], lhsT=ones[:], rhs=u2T[:, g, :n_out], start=(g == 0), stop=(g == GC - 1))
                return e.matmul(pbank[5][:, :n_out], lhsT=ones[:], rhs=sq[:, :n_out], start=(g == 0), stop=(g == GC - 1))
            pend_stats.append((mmst, [u2_b[g], sqb, cb]))
            if len(pend_stats) > 1:
                f_, r_ = pend_stats.pop(0)
                P.op("pe", f_, reads=r_, writes=[pbuf[4], pbuf[5]])
        for f_, r_ in pend_stats:
            P.op("pe", f_, reads=r_, writes=[pbuf[4], pbuf[5]])
        DCF = D // 2
        P.op("act", lambda e: e.activation(out=mu[:, :n_out], in_=pbank[4][:, :n_out], func=AF.Copy, scale=1.0 / DCF), reads=[pbuf[4]], writes=[mu_b])
        m2, m2b = tmpp.get()
        P.op("dve", lambda e, m2=m2: e.tensor_tensor(out=m2[:, :n_out], in0=mu[:, :n_out], in1=mu[:, :n_out], op=ALU.mult), reads=[mu_b], writes=[m2b])
        P.op("dve", lambda e, m2=m2: e.scalar_tensor_tensor(out=m2[:, :n_out], in0=pbank[5][:, :n_out], scalar=1.0 / DCF, in1=m2[:, :n_out], op0=ALU.mult, op1=ALU.subtract),
             reads=[pbuf[5], m2b], writes=[m2b])
        P.op("act", lambda e, m2=m2: e.activation(out=rstd[:, :n_out], in_=m2[:, :n_out], func=AF.Sqrt, bias=epsl[:], scale=1.0), reads=[m2b, cb], writes=[rstd_b])
        P.op("dve", lambda e: e.reciprocal(out=rstd[:, :n_out], in_=rstd[:, :n_out]), reads=[rstd_b], writes=[rstd_b])
        for g in range(GC):
            t1, t1b = tmpp.get()
            P.op("dve", lambda e, t1=t1, g=g: e.tensor_tensor(out=t1[:, :n_out], in0=u2T[:, g, :n_out], in1=mu[:, :n_out], op=ALU.subtract), reads=[u2_b[g], mu_b], writes=[t1b])
            P.op("dve", lambda e, t1=t1: e.tensor_tensor(out=t1[:, :n_out], in0=t1[:, :n_out], in1=rstd[:, :n_out], op=ALU.mult), reads=[t1b, rstd_b], writes=[t1b])
            P.op("act", lambda e, t1=t1, g=g: e.activation(out=yT[:, GC + g, :n_out], in_=t1[:, :n_out], func=AF.Silu, scale=V(f"cfg{l}", g), bias=V(f"cfbeta{l}", g)),
                 reads=[t1b, cb], writes=[yT_b])
        pre = resid_load(0, c0, n_out)
        for dc in range(KC):
            t, b = load_w(W_out[l], dc)
            pb = 6 + dc % 2
            def mm(e, t=t, pb=pb):
                for kc in range(KC):
                    ins = e.matmul(pbank[pb][:, :n_out], lhsT=t[:, kc, :], rhs=yT[:, kc, :n_out], start=(kc == 0), stop=(kc == KC - 1))
                return ins
            P.op("pe", mm, reads=[b, yT_b], writes=[pbuf[pb]])
            nxt = resid_load(dc + 1, c0, n_out) if dc + 1 < KC else None
            resid_fin(l, 2, dc, c0, n_out, pb, None, pre)
            pre = nxt

    def resid_load(dc, c0, n):
        xc, xb = xcp.get()
        P.op("act", lambda e: e.dma_start(out=xc[:, :n], in_=xT[dc, :, c0:c0 + n]), reads=xT_bufs(c0, n), writes=[xb], dma=True)
        return xc, xb

    def resid_fin(l, gi, dc, c0, n, pb, gmask, pre):
        xc, xb = pre
        gcol = mods[l][:, gi * KC + dc:gi * KC + dc + 1]
        if gmask is None:
            P.op("dve", lambda e: e.scalar_tensor_tensor(out=xc[:, :n], in0=pbank[pb][:, :n], scalar=gcol, in1=xc[:, :n], op0=ALU.mult, op1=ALU.add),
                 reads=[pbuf[pb], xb, mod_b[l]], writes=[xb])
        else:
            tm, tmb = tmpp.get()
            P.op("dve", lambda e: e.tensor_tensor(out=tm[:, :n], in0=pbank[pb][:, :n], in1=gmask, op=ALU.mult), reads=[pbuf[pb], Gbc_b], writes=[tmb])
            P.op("dve", lambda e: e.scalar_tensor_tensor(out=xc[:, :n], in0=tm[:, :n], scalar=gcol, in1=xc[:, :n], op0=ALU.mult, op1=ALU.add),
                 reads=[tmb, xb, mod_b[l]], writes=[xb])
        P.op("act", lambda e: e.dma_start(out=xT[dc, :, c0:c0 + n], in_=xc[:, :n]), reads=[xb], writes=xT_bufs(c0, n), dma=True)

    sa_p = Pool_(2, [128, BLK], F32, "sa")
    def ffn_part(l, c0, n, wg, wu, wd, f0, f1, gmask):
        nf = f1 - f0
        for fc in range(f0, f1):
            tg, bg = load_w(wg, fc)
            tu, bu = load_w(wu, fc)
            def mm(e, tg=tg, tu=tu):
                for kc in range(KC):
                    e.matmul(pbank[0][:, :n], lhsT=tg[:, kc, :], rhs=hT[:, kc, :n], start=(kc == 0), stop=(kc == KC - 1))
                for kc in range(KC):
                    ins = e.matmul(pbank[1][:, :n], lhsT=tu[:, kc, :], rhs=hT[:, kc, :n], start=(kc == 0), stop=(kc == KC - 1))
                return ins
            pa, pu = (0, 1) if (fc - f0) % 2 == 0 else (2, 3)
            def mm2(e, tg=tg, tu=tu, pa=pa, pu=pu):
                for kc in range(KC):
                    e.matmul(pbank[pa][:, :n], lhsT=tg[:, kc, :], rhs=hT[:, kc, :n], start=(kc == 0), stop=(kc == KC - 1))
                for kc in range(KC):
                    ins = e.matmul(pbank[pu][:, :n], lhsT=tu[:, kc, :], rhs=hT[:, kc, :n], start=(kc == 0), stop=(kc == KC - 1))
                return ins
            P.op("pe", mm2, reads=[bg, bu, hT_b], writes=[pbuf[pa], pbuf[pu]])
            sa, sab = sa_p.get()
            P.op("act", lambda e, sa=sa, pa=pa: e.activation(out=sa[:, :n], in_=pbank[pa][:, :n], func=AF.Silu), reads=[pbuf[pa]], writes=[sab])
            P.op("dve", lambda e, sa=sa, pu=pu, fc=fc: e.tensor_tensor(out=hb[:, fc - f0, :n], in0=pbank[pu][:, :n], in1=sa[:, :n], op=ALU.mult),
                 reads=[pbuf[pu], sab], writes=[hb_b])
        pre = resid_load(0, c0, n)
        for dc in range(KC):
            t, b = wpool.get()
            P.op("sp", lambda e, t=t, dc=dc: e.dma_start(out=t[:, :nf, :], in_=wd.dst[dc][:, f0:f1, :]), reads=[wd.bufs[dc]], writes=[b], dma=True)
            pb = 6 + dc % 2
            def mm(e, t=t, pb=pb):
                for k in range(nf):
                    ins = e.matmul(pbank[pb][:, :n], lhsT=t[:, k, :], rhs=hb[:, k, :n], start=(k == 0), stop=(k == nf - 1))
                return ins
            P.op("pe", mm, reads=[b, hb_b], writes=[pbuf[pb]])
            nxt = resid_load(dc + 1, c0, n) if dc + 1 < KC else None
            resid_fin(l, 5, dc, c0, n, pb, gmask, pre)
            pre = nxt

    def split(n, parts):
        q, r = divmod(n, parts)
        res, o = [], 0
        for i in range(parts):
            s = q + (1 if i < r else 0)
            if s:
                res.append((o, o + s))
            o += s
        return res

    lg = sb([128, NSUB, NE], F32, "lg"); lg2 = sb([128, NSUB, NE], F32, "lg2")
    mk1 = sb([128, NSUB, NE], F32, "mk1"); mk2 = sb([128, NSUB, NE], F32, "mk2"); G = sb([128, NSUB, NE], F32, "G")
    m1 = sb([128, NSUB], F32, "m1"); m2_ = sb([128, NSUB], F32, "m2"); g1 = sb([128, NSUB], F32, "g1"); g2 = sb([128, NSUB], F32, "g2")
    GT = sb([NE, BLK], F32, "GT")
    rt_b = Buf("router")

    def ffn(l, c0, n):
        moe = (l % 2 == 1)
        norm_mod(l, 1, c0, n, router=moe)
        if not moe:
            for (f0, f1) in split(FC, 3):
                ffn_part(l, c0, n, W_fg, W_fu, W_fd, f0, f1, None)
            return
        for s in range(NSUB):
            P.op("dve", lambda e, s=s: e.tensor_copy(out=lg[:, s, :], in_=pbank[s][:, 0:NE]), reads=[pbuf[s]], writes=[rt_b])
        P.op("dve", lambda e: e.tensor_reduce(out=m1[:], in_=lg[:], axis=AX.X, op=ALU.max), reads=[rt_b], writes=[rt_b])
        for s in range(NSUB):
            P.op("dve", lambda e, s=s: e.tensor_scalar(out=mk1[:, s, :], in0=lg[:, s, :], scalar1=m1[:, s:s + 1], scalar2=None, op0=ALU.is_equal), reads=[rt_b], writes=[rt_b])
        P.op("dve", lambda e: e.scalar_tensor_tensor(out=lg2[:], in0=mk1[:], scalar=-1e30, in1=lg[:], op0=ALU.mult, op1=ALU.add), reads=[rt_b], writes=[rt_b])
        P.op("dve", lambda e: e.tensor_reduce(out=m2_[:], in_=lg2[:], axis=AX.X, op=ALU.max), reads=[rt_b], writes=[rt_b])
        for s in range(NSUB):
            P.op("dve", lambda e, s=s: e.tensor_scalar(out=mk2[:, s, :], in0=lg2[:, s, :], scalar1=m2_[:, s:s + 1], scalar2=None, op0=ALU.is_equal), reads=[rt_b], writes=[rt_b])
        P.op("dve", lambda e: e.tensor_tensor(out=g2[:], in0=m2_[:], in1=m1[:], op=ALU.subtract), reads=[rt_b], writes=[rt_b])
        P.op("act", lambda e: e.activation(out=g2[:], in_=g2[:], func=AF.Exp), reads=[rt_b], writes=[rt_b])
        P.op("dve", lambda e: e.tensor_scalar(out=g1[:], in0=g2[:], scalar1=1.0, scalar2=None, op0=ALU.add), reads=[rt_b], writes=[rt_b])
        P.op("dve", lambda e: e.reciprocal(out=g1[:], in_=g1[:]), reads=[rt_b], writes=[rt_b])
        P.op("dve", lambda e: e.tensor_tensor(out=g2[:], in0=g2[:], in1=g1[:], op=ALU.mult), reads=[rt_b], writes=[rt_b])
        for s in range(NSUB):
            P.op("dve", lambda e, s=s: e.tensor_scalar(out=G[:, s, :], in0=mk1[:, s, :], scalar1=g1[:, s:s + 1], scalar2=None, op0=ALU.mult), reads=[rt_b], writes=[rt_b])
            P.op("dve", lambda e, s=s: e.scalar_tensor_tensor(out=G[:, s, :], in0=mk2[:, s, :], scalar=g2[:, s:s + 1], in1=G[:, s, :], op0=ALU.mult, op1=ALU.add), reads=[rt_b], writes=[rt_b])
        for s in range(NSUB):
            P.op("pe", lambda e, s=s: e.transpose(pbank[4][0:NE, s * 128:(s + 1) * 128], G[:, s, :], ident[:]), reads=[rt_b, cb], writes=[pbuf[4]])
        P.op("act", lambda e: e.activation(out=GT[:], in_=pbank[4][0:NE, :n], func=AF.Copy), reads=[pbuf[4]], writes=[rt_b])
        for ex in range(NE):
            pb = 4 + ex % 2
            P.op("pe", lambda e, ex=ex, pb=pb: e.matmul(pbank[pb][:, :n], lhsT=sel[:, ex * 128:(ex + 1) * 128], rhs=GT[:, :n], start=True, stop=True), reads=[rt_b, cb], writes=[pbuf[pb]])
            P.op("act", lambda e, ex=ex, pb=pb: e.activation(out=Gbc[:, 0, :], in_=pbank[pb][:, :n], func=AF.Copy), reads=[pbuf[pb]], writes=[Gbc_b])
            for (f0, f1) in split(FE, 1):
                ffn_part(l, c0, n, W_mg[ex], W_mu[ex], W_md[ex], f0, f1, Gbc[:, 0, :])

    def final(tt):
        c0, n = tt * 128, 128
        rms_stats(c0, n, [4])
        xn = SCR[:, 0:D].rearrange("p (k c) -> p k c", c=128)
        ot = SCR[:, D:2 * D]
        for kc in range(KC):
            xc, xb = xcp.get()
            P.op("act", lambda e, xc=xc, kc=kc: e.dma_start(out=xc[:, :n], in_=xT[kc, :, c0:c0 + n]), reads=xT_bufs(c0, n), writes=[xb], dma=True)
            P.op("dve", lambda e, xc=xc, kc=kc: e.scalar_tensor_tensor(out=xn[:, kc, :], in0=xc[:, :n], scalar=V("fg", kc), in1=rstd[:, :n], op0=ALU.mult, op1=ALU.mult),
                 reads=[xb, rstd_b, cb], writes=[scr_b[0]])
        for c4 in range(KC // 4):
            pb = c4 % 4
            def tr(e, c4=c4, pb=pb):
                for q in range(4):
                    ins = e.transpose(pbank[pb][:, q * 128:(q + 1) * 128], xn[:, c4 * 4 + q, :], ident[:])
                return ins
            P.op("pe", tr, reads=[scr_b[0], cb], writes=[pbuf[pb]])
            if c4 % 2 == 0:
                P.op("act", lambda e, c4=c4, pb=pb: e.activation(out=ot[:, c4 * 512:(c4 + 1) * 512], in_=pbank[pb][:], func=AF.Copy), reads=[pbuf[pb]], writes=[scr_b[1]])
            else:
                P.op("dve", lambda e, c4=c4, pb=pb: e.tensor_copy(out=ot[:, c4 * 512:(c4 + 1) * 512], in_=pbank[pb][:]), reads=[pbuf[pb]], writes=[scr_b[1]])
        return [P.op("act", lambda e: e.dma_start(out=out_d[c0 - HALO:c0 - HALO + 128, :], in_=ot), reads=[scr_b[1]], dma=True)]

    main = [(HALO + i * BLK, BLK) for i in range(NB)]
    for l in range(L):
        for i in reversed(range(NB)):
            c0, n = main[i]
            mixer(l, c0, n, HALO, mask=(i == 0))
        if l + 1 < L:
            mixer(l, 0, HALO, 0, mask=False)
            ffn(l, 0, HALO)
        for (c0, n) in main:
            ffn(l, c0, n)
    alltoks = []
    for tt in range(1, T // 128):
        alltoks += final(tt)
    P.final_wait("act", alltoks[-12:])
    P.emit_all()
    P.close()
    es.close()
    return nc


def _pack_inputs(cfg, b, inp, hm):
    KC, GC, NE, L = cfg.KC, cfg.GC, cfg.NE, cfg.L
    vecs = np.zeros((128, cfg.NV), np.float32)
    def put(name, arr):
        o, n = cfg.off[name]
        a = np.asarray(arr, np.float32)
        if a.ndim == 1:
            vecs[:, o:o + n] = a.reshape(-1, 128).T
        else:
            k = a.shape[0]
            vecs[:, o:o + n] = a.reshape(k, -1, 128).transpose(2, 0, 1).reshape(128, -1)
    for l in range(L):
        put(f"gmix{l}", inp["norm_mix_g"][l]); put(f"gffn{l}", inp["norm_ffn_g"][l]); put(f"bada{l}", inp["b_ada"][l])
        put(f"scw{l}", inp["sc_conv_w"][l]); put(f"cfw{l}", inp["cf_conv_w"][l])
        put(f"cfb{l}", inp["cf_conv_b"][l]); put(f"cfg{l}", inp["cf_ln_g"][l]); put(f"cfbeta{l}", inp["cf_ln_b"][l])
    put("fg", inp["final_g"]); put("c", inp["c"][b])
    o, n = cfg.off["hmask"]
    vecs[:, o] = hm
    o, n = cfg.off["rw"]
    rw = np.asarray(inp["router_w"][0], np.float32)
    vecs[:, o:o + n] = rw.reshape(KC, 128, NE).transpose(1, 0, 2).reshape(128, KC * NE)
    return vecs


_CACHE = {}


def run(cfg, inputs):
    key = (cfg.D, cfg.DFF, cfg.DFE, cfg.S, cfg.BLK, cfg.NCPB)
    if key not in _CACHE:
        _CACHE[key] = build(cfg)
    nc = _CACHE[key]
    NE = cfg.NE
    sel = np.zeros((NE, NE * 128), np.float32)
    for e in range(NE):
        sel[e, e * 128:(e + 1) * 128] = 1.0
    B = inputs["x"].shape[0]
    in_maps = []
    f = lambda a: np.ascontiguousarray(np.asarray(a, np.float32))
    shared = dict(
        ident=np.eye(128, dtype=np.float32), ones=np.ones((128, 128), np.float32), sel=sel,
        w_ada=f(inputs["w_ada"]), w_in=f(inputs["w_in"]), w_out=f(inputs["w_out"]),
        ffn_w_gate=f(inputs["ffn_w_gate"][0]), ffn_w_up=f(inputs["ffn_w_up"][0]), ffn_w_down=f(inputs["ffn_w_down"][0]),
        moe_w_gate=f(inputs["moe_w_gate"][0]), moe_w_up=f(inputs["moe_w_up"][0]), moe_w_down=f(inputs["moe_w_down"][0]),
    )
    NCPB, TM, T = cfg.NCPB, cfg.TM, cfg.T
    x = np.asarray(inputs["x"], np.float32)
    for b in range(B):
        for j in range(NCPB):
            m = dict(shared)
            xs = np.zeros((T, cfg.D), np.float32)
            lo = j * TM
            if j > 0:
                xs[:] = x[b, lo - 128:lo + TM]
            else:
                xs[128:] = x[b, 0:TM]
            m["x"] = xs
            m["vecs"] = _pack_inputs(cfg, b, inputs, 0.0 if j == 0 else 1.0)
            in_maps.append(m)
    ncores = B * NCPB
    res = run_bass_kernel_spmd(nc, in_maps, core_ids=list(range(ncores)))
    out = np.empty((B, cfg.S, cfg.D), np.float32)
    for b in range(B):
        for j in range(NCPB):
            out[b, j * TM:(j + 1) * TM] = res.results[b * NCPB + j]["out"]
    return out


def kernel(**inputs):
    cfg = Cfg()
    return run(cfg, inputs)
```

```python
import numpy as np
from contextlib import ExitStack
import concourse.bass as bass
import concourse.mybir as mybir
from concourse.bass_utils import run_bass_kernel_spmd

F32 = mybir.dt.float32
BF16 = mybir.dt.bfloat16
AF = mybir.ActivationFunctionType
ALU = mybir.AluOpType
AX = mybir.AxisListType

SEM_EPOCH = 30000


class Buf:
    __slots__ = ("name", "last_write", "reads")

    def __init__(self, name=""):
        self.name = name
        self.last_write = None
        self.reads = []


class Prog:
    ENG = ("pe", "act", "dve", "pool", "sp")

    def __init__(self, nc, ndma_sems=6):
        self.nc = nc
        self.ops = {e: [] for e in self.ENG}
        self.sems = []
        self._sem_ctx = []
        self.cur = {}
        self.waited = {e: {} for e in self.ENG}
        self.dma_pool = {}
        self.dma_rr = {e: 0 for e in self.ENG}
        self.ndma = ndma_sems

    def _new_sem(self, name):
        cm = self.nc.semaphore(name)
        h = cm.__enter__()
        self._sem_ctx.append(cm)
        self.sems.append(h)
        return len(self.sems) - 1

    def _compute_token(self, eng):
        st = self.cur.get(eng)
        if st is None or st[1] >= SEM_EPOCH:
            st = [self._new_sem(f"s_{eng}_{len(self.sems)}"), 0]
            self.cur[eng] = st
        st[1] += 1
        return (st[0], st[1])

    def _dma_token(self, q):
        pool = self.dma_pool.setdefault(q, [])
        if len(pool) < self.ndma:
            pool.append([self._new_sem(f"d_{q}_{len(pool)}"), 0, None])
        i = self.dma_rr[q] % self.ndma
        self.dma_rr[q] += 1
        ent = pool[i]
        prev = ent[2]
        if ent[1] >= SEM_EPOCH * 16:
            ent[0] = self._new_sem(f"d_{q}_{i}_{len(self.sems)}")
            ent[1] = 0
        ent[1] += 16
        tok = (ent[0], ent[1])
        ent[2] = tok
        return tok, prev

    def op(self, eng, emit, reads=(), writes=(), dma=False):
        deps = set()
        for b in reads:
            if b.last_write is not None:
                deps.add(b.last_write)
        for b in writes:
            if b.last_write is not None:
                deps.add(b.last_write)
            for t in b.reads:
                deps.add(t)
        if dma:
            tok, prev = self._dma_token(eng)
            if prev is not None:
                deps.add(prev)
            inc = 16
        else:
            tok = self._compute_token(eng)
            inc = 1
        own = self.cur[eng][0] if (not dma and eng in self.cur) else None
        w = self.waited[eng]
        need = {}
        for (s, v) in deps:
            if eng == "pe" and not dma and s == own:
                continue
            if need.get(s, 0) < v:
                need[s] = v
        waits = []
        for s, v in need.items():
            if w.get(s, 0) >= v:
                continue
            w[s] = v
            waits.append((s, v))
        self.ops[eng].append((waits, emit, tok, inc))
        for b in reads:
            b.reads.append(tok)
        for b in writes:
            b.last_write = tok
            b.reads = []
        return tok

    def final_wait(self, eng, toks):
        self.ops[eng].append((list(toks), None, None, 0))

    def emit_all(self):
        nc, sems, ops = self.nc, self.sems, self.ops
        with nc.Block() as block:
            def run(engname):
                def body(e):
                    for (waits, emit, tok, inc) in ops[engname]:
                        for (s, v) in waits:
                            e.wait_ge(sems[s], v)
                        if emit is not None:
                            emit(e).then_inc(sems[tok[0]], inc)
                return body
            block.sync(run("sp"))
            block.scalar(run("act"))
            block.vector(run("dve"))
            block.gpsimd(run("pool"))
            block.tensor(run("pe"))

    def close(self):
        for cm in reversed(self._sem_ctx):
            cm.__exit__(None, None, None)


class Cfg:
    def __init__(self, D=4096, DFF=11008, DFE=4096, NE=8, S=8192, BLK=512, L=2, NCPB=4):
        self.D, self.DFF, self.DFE, self.NE, self.S, self.BLK, self.L = D, DFF, DFE, NE, S, BLK, L
        self.NCPB = NCPB
        self.TM = S // NCPB
        self.T = self.TM + 128
        self.KC = D // 128
        self.GC = D // 256
        self.FC = DFF // 128
        self.FE = DFE // 128
        self.NB = self.TM // BLK
        KC, GC = self.KC, self.GC
        off = {}
        c = 0
        def add(name, n):
            nonlocal c
            off[name] = (c, n)
            c += n
        for l in range(L):
            add(f"gmix{l}", KC); add(f"gffn{l}", KC); add(f"bada{l}", 6 * KC)
            add(f"scw{l}", 3 * GC); add(f"cfw{l}", 31 * GC)
            add(f"cfb{l}", GC); add(f"cfg{l}", GC); add(f"cfbeta{l}", GC)
        add("fg", KC); add("c", KC); add("rw", KC * NE); add("hmask", 1)
        self.off = off
        self.NV = c


def build(cfg):
    D, KC, GC, FC, FE, NE, S, BLK, L, NB = cfg.D, cfg.KC, cfg.GC, cfg.FC, cfg.FE, cfg.NE, cfg.S, cfg.BLK, cfg.L, cfg.NB
    DFF, DFE = cfg.DFF, cfg.DFE
    HALO = 128
    T, TM = cfg.T, cfg.TM
    NSUB = BLK // 128
    nc = bass.Bass("TRN2", target_bir_lowering=False)
    def din(name, shape):
        return nc.dram_tensor(name, shape, F32, kind="ExternalInput").ap()
    x_in = din("x", [T, D])
    vecs_d = din("vecs", [128, cfg.NV])
    ident_d = din("ident", [128, 128])
    ones_d = din("ones", [128, 128])
    sel_d = din("sel", [NE, NE * 128])
    w_ada = din("w_ada", [L, D, 6 * D])
    w_in = din("w_in", [L, D, 5 * D // 2])
    w_out = din("w_out", [L, D, D])
    f_gate = din("ffn_w_gate", [D, DFF])
    f_up = din("ffn_w_up", [D, DFF])
    f_down = din("ffn_w_down", [DFF, D])
    m_gate = din("moe_w_gate", [NE, D, DFE])
    m_up = din("moe_w_up", [NE, D, DFE])
    m_down = din("moe_w_down", [NE, DFE, D])
    out_d = nc.dram_tensor("out", [TM, D], F32, kind="ExternalOutput").ap()
    xT = nc.dram_tensor("xT_scr", [KC, 128, T], F32).ap()

    es = ExitStack()
    P = Prog(nc)
    cnt = [0]
    def sb(shape, dt, name=None):
        cnt[0] += 1
        return es.enter_context(nc.sbuf_tensor(name or f"t{cnt[0]}", shape, dt))
    class Pool_:
        def __init__(self, n, shape, dt, name):
            self.t = [sb(shape, dt, f"{name}{i}") for i in range(n)]
            self.b = [Buf(f"{name}{i}") for i in range(n)]
            self.i = 0
        def get(self):
            k = self.i % len(self.t)
            self.i += 1
            return self.t[k], self.b[k]
    NWMAX = BLK + HALO
    ident = sb([128, 128], F32, "ident_s"); ones = sb([128, 128], F32, "ones_s")
    vecs = sb([128, cfg.NV], F32, "vecs_s"); sel = sb([NE, NE * 128], F32, "sel_s")
    epsr = sb([128, 1], F32, "epsr"); epsl = sb([128, 1], F32, "epsl")
    cb = Buf("const")
    P.op("sp", lambda e: e.dma_start(out=ident[:], in_=ident_d), writes=[cb], dma=True)
    P.op("sp", lambda e: e.dma_start(out=ones[:], in_=ones_d), writes=[cb], dma=True)
    P.op("sp", lambda e: e.dma_start(out=vecs[:], in_=vecs_d), writes=[cb], dma=True)
    P.op("sp", lambda e: e.dma_start(out=sel[:], in_=sel_d), writes=[cb], dma=True)
    P.op("dve", lambda e: e.memset(epsr[:], 1e-6), writes=[cb])
    P.op("dve", lambda e: e.memset(epsl[:], 1e-5), writes=[cb])
    def V(name, j=None, n=1):
        o, ln = cfg.off[name]
        if j is None:
            return vecs[:, o:o + ln]
        return vecs[:, o + j:o + j + n]
    pbank = [es.enter_context(nc.psum_tensor(f"ps{i}", [128, 512], F32)) for i in range(8)]
    pbuf = [Buf(f"ps{i}") for i in range(8)]
    wpool = Pool_(4, [128, KC, 128], BF16, "w")
    xcp = Pool_(3, [128, NWMAX], F32, "xc")
    tmpp = Pool_(4, [128, NWMAX], F32, "tmp")
    hT = sb([128, KC, NWMAX], BF16, "hT"); hT_b = Buf("hT")
    yT = sb([128, KC, BLK], BF16, "yT"); yT_b = Buf("yT")
    SCR = sb([128, 2 * D], F32, "SCR"); scr_b = [Buf("scr0"), Buf("scr1")]
    assert GC * BLK <= 2 * D
    u2T = SCR[:, 0:GC * BLK].rearrange("p (g t) -> p g t", g=GC)
    u2_b = [scr_b[(g * BLK) // D] for g in range(GC)]
    class ScrPool:
        def __init__(self, view):
            self.i = 0; self.view = view
        def get(self):
            k = self.i % 2; self.i += 1
            return self.view(SCR[:, k * D:(k + 1) * D]), scr_b[k]
    wapool = ScrPool(lambda a: a.rearrange("p (k c) -> p k c", c=512))
    xin_p = ScrPool(lambda a: a)
    NHB = max((FC + 2) // 3, FE)
    assert NHB <= KC
    hb = yT; hb_b = yT_b
    rstd = sb([128, NWMAX], F32, "rstd"); rstd_b = Buf("rstd")
    mu = sb([128, BLK], F32, "mu"); mu_b = Buf("mu")
    Gbc = sb([128, 1, BLK], F32, "Gbc"); Gbc_b = Buf("Gbc")
    mods = [sb([128, 6 * KC], F32, f"mod{l}") for l in range(L)]; mod_b = [Buf() for _ in range(L)]
    gs = [[sb([128, KC], F32, f"gs{l}_{i}") for i in range(2)] for l in range(L)]
    cact = sb([128, KC], F32, "cact")

    class WT:
        def __init__(self, name, src, K, N):
            self.src, self.kch, self.nt = src, K // 128, N // 128
            self.dst = nc.dram_tensor("wb_" + name, [self.nt, 128, self.kch, 128], BF16).ap()
            self.bufs = [Buf(f"{name}{j}") for j in range(self.nt)]
        def prepass(self, js=None):
            for j in (range(self.nt) if js is None else js):
                P.op("pool", lambda e, j=j: e.dma_start(out=self.dst[j], in_=wtile_view(self.src, j)), writes=[self.bufs[j]], dma=True)
    W_in = [WT(f"in{l}", w_in[l], D, 5 * D // 2) for l in range(L)]
    W_out = [WT(f"out{l}", w_out[l], D, D) for l in range(L)]
    W_fg, W_fu, W_fd = WT("fg", f_gate, D, DFF), WT("fu", f_up, D, DFF), WT("fd", f_down, DFF, D)
    W_mg = [WT(f"mg{e}", m_gate[e], D, DFE) for e in range(NE)]
    W_mu = [WT(f"mu{e}", m_up[e], D, DFE) for e in range(NE)]
    W_md = [WT(f"md{e}", m_down[e], DFE, D) for e in range(NE)]

    def wtile_view(wap, j):
        return wap.rearrange("(kc p) n -> p kc n", p=128)[:, :, j * 128:(j + 1) * 128]

    def load_w(wt, j):
        t, b = wpool.get()
        P.op("sp", lambda e: e.dma_start(out=t[:], in_=wt.dst[j]), reads=[wt.bufs[j]], writes=[b], dma=True)
        return t, b

    def ntiles(n):
        r = []
        o = 0
        while o < n:
            r.append((o, min(512, n - o)))
            o += 512
        return r

    P.op("act", lambda e: e.activation(out=cact[:], in_=V("c"), func=AF.Silu), reads=[cb], writes=[cb])
    KQ = KC // 4
    for l in range(L):
        wav = w_ada[l].rearrange("(kc p) n -> p kc n", p=128)
        for grp in range(6 * KC // 4):
            for st in range(4):
                t, b = wapool.get()
                P.op("sp", lambda e, t=t, st=st, grp=grp, wav=wav: e.dma_start(out=t[:, 0:KQ, :], in_=wav[:, st * KQ:(st + 1) * KQ, grp * 512:(grp + 1) * 512]), writes=[b], dma=True)
                def mm(e, t=t, st=st):
                    for c in range(4):
                        for kq in range(KQ):
                            ins = e.matmul(pbank[c][:, 0:1], lhsT=t[:, kq, c * 128:(c + 1) * 128], rhs=cact[:, st * KQ + kq:st * KQ + kq + 1],
                                           start=(st == 0 and kq == 0), stop=(st == 3 and kq == KQ - 1))
                    return ins
                P.op("pe", mm, reads=[b, cb], writes=[pbuf[c] for c in range(4)])
            for c in range(4):
                j = grp * 4 + c
                P.op("dve", lambda e, l=l, j=j, c=c: e.tensor_tensor(out=mods[l][:, j:j + 1], in0=pbank[c][:, 0:1], in1=V(f"bada{l}", j), op=ALU.add),
                     reads=[pbuf[c], cb], writes=[mod_b[l]])
        P.op("dve", lambda e, l=l: e.scalar_tensor_tensor(out=gs[l][0][:], in0=mods[l][:, KC:2 * KC], scalar=1.0, in1=V(f"gmix{l}"), op0=ALU.add, op1=ALU.mult),
             reads=[mod_b[l], cb], writes=[mod_b[l]])
        P.op("dve", lambda e, l=l: e.scalar_tensor_tensor(out=gs[l][1][:], in0=mods[l][:, 4 * KC:5 * KC], scalar=1.0, in1=V(f"gffn{l}"), op0=ALU.add, op1=ALU.mult),
             reads=[mod_b[l], cb], writes=[mod_b[l]])

    for l in range(L):
        W_in[l].prepass(); W_out[l].prepass()
        if l % 2 == 0:
            for fc in range(FC):
                W_fg.prepass([fc]); W_fu.prepass([fc])
            W_fd.prepass()
        else:
            for ex in range(NE):
                for fc in range(FE):
                    W_mg[ex].prepass([fc]); W_mu[ex].prepass([fc])
                W_md[ex].prepass()
    xT_b = [Buf(f"xT{b}") for b in range(T // 128)]

    xts_p = Pool_(2, [128, 4, 128], F32, "xts")
    for tt in range(T // 128):
        blk = tt
        t, b = xin_p.get()
        P.op("act", lambda e, t=t, tt=tt: e.dma_start(out=t[:], in_=x_in[tt * 128:(tt + 1) * 128, :]), writes=[b], dma=True)
        for c4 in range(KC // 4):
            pb = c4 % 4
            def tr(e, t=t, c4=c4, pb=pb):
                for q in range(4):
                    ins = e.transpose(pbank[pb][:, q * 128:(q + 1) * 128], t[:, (c4 * 4 + q) * 128:(c4 * 4 + q + 1) * 128], ident[:])
                return ins
            P.op("pe", tr, reads=[b, cb], writes=[pbuf[pb]])
            ts_, tb_ = xts_p.get()
            eng = "act" if c4 % 2 == 0 else "dve"
            if eng == "act":
                P.op("act", lambda e, ts_=ts_, pb=pb: e.activation(out=ts_[:].rearrange("p a b -> p (a b)"), in_=pbank[pb][:], func=AF.Copy), reads=[pbuf[pb]], writes=[tb_])
            else:
                P.op("dve", lambda e, ts_=ts_, pb=pb: e.tensor_copy(out=ts_[:].rearrange("p a b -> p (a b)"), in_=pbank[pb][:]), reads=[pbuf[pb]], writes=[tb_])
            P.op("act", lambda e, ts_=ts_, c4=c4, tt=tt: e.dma_start(out=xT[c4 * 4:(c4 + 1) * 4].rearrange("c p t -> p c t")[:, :, tt * 128:(tt + 1) * 128], in_=ts_[:]),
                 reads=[tb_], writes=[xT_b[blk]], dma=True)

    def xT_bufs(c0, n):
        return [xT_b[b] for b in range(c0 // 128, (c0 + n - 1) // 128 + 1)]

    def xload(kc, c0, n):
        xc, xb = xcp.get()
        P.op("act", lambda e: e.dma_start(out=xc[:, :n], in_=xT[kc, :, c0:c0 + n]), reads=xT_bufs(c0, n), writes=[xb], dma=True)
        return xc, xb

    def rms_stats(c0, n, pbs):
        tl = ntiles(n)
        pend = [xload(kc, c0, n) for kc in range(min(2, KC))]
        for kc in range(KC):
            if kc + 2 < KC:
                pend.append(xload(kc + 2, c0, n))
            xc, xb = pend.pop(0)
            sq, sqb = tmpp.get()
            P.op("act", lambda e, xc=xc, sq=sq: e.activation(out=sq[:, :n], in_=xc[:, :n], func=AF.Square), reads=[xb], writes=[sqb])
            def mm(e, sq=sq, kc=kc):
                for i, (o, w) in enumerate(tl):
                    ins = e.matmul(pbank[pbs[i]][:, :w], lhsT=ones[:], rhs=sq[:, o:o + w], start=(kc == 0), stop=(kc == KC - 1))
                return ins
            P.op("pe", mm, reads=[sqb, cb], writes=[pbuf[pbs[i]] for i in range(len(tl))])
        for i, (o, w) in enumerate(tl):
            P.op("act", lambda e, i=i, o=o, w=w: e.activation(out=rstd[:, o:o + w], in_=pbank[pbs[i]][:, :w], func=AF.Sqrt, scale=1.0 / D, bias=epsr[:]),
                 reads=[pbuf[pbs[i]], cb], writes=[rstd_b])
        P.op("dve", lambda e: e.reciprocal(out=rstd[:, :n], in_=rstd[:, :n]), reads=[rstd_b], writes=[rstd_b])

    def norm_mod(l, which, c0, n, router=False):
        rms_stats(c0, n, [4, 5])
        shift = mods[l][:, (0 if which == 0 else 3 * KC):]
        gsv = gs[l][which]
        pend = [xload(kc, c0, n) for kc in range(min(2, KC))]
        for kc in range(KC):
            if kc + 2 < KC:
                pend.append(xload(kc + 2, c0, n))
            xc, xb = pend.pop(0)
            tm, tmb = tmpp.get()
            P.op("dve", lambda e, xc=xc, tm=tm: e.tensor_tensor(out=tm[:, :n], in0=xc[:, :n], in1=rstd[:, :n], op=ALU.mult), reads=[xb, rstd_b], writes=[tmb])
            if not router:
                P.op("act", lambda e, tm=tm, kc=kc: e.activation(out=hT[:, kc, :n], in_=tm[:, :n], func=AF.Identity, scale=gsv[:, kc:kc + 1], bias=shift[:, kc:kc + 1]),
                     reads=[tmb, mod_b[l]], writes=[hT_b])
            else:
                hf, hfb = tmpp.get()
                P.op("act", lambda e, tm=tm, kc=kc, hf=hf: e.activation(out=hf[:, :n], in_=tm[:, :n], func=AF.Identity, scale=gsv[:, kc:kc + 1], bias=shift[:, kc:kc + 1]),
                     reads=[tmb, mod_b[l]], writes=[hfb])
                P.op("dve", lambda e, kc=kc, hf=hf: e.tensor_copy(out=hT[:, kc, :n], in_=hf[:, :n]), reads=[hfb], writes=[hT_b])
                def mm(e, hf=hf, kc=kc):
                    for s in range(n // 128):
                        ins = e.matmul(pbank[s][:, 0:NE], lhsT=hf[:, s * 128:(s + 1) * 128], rhs=V("rw")[:, kc * NE:(kc + 1) * NE], start=(kc == 0), stop=(kc == KC - 1))
                    return ins
                P.op("pe", mm, reads=[hfb, cb], writes=[pbuf[s] for s in range(n // 128)])

    zc_p = Pool_(2, [128, NWMAX], F32, "zc")
    acc_p = Pool_(2, [128, BLK], F32, "acc")
    def inproj(l, j, nw, pbs, lo=0):
        t, b = load_w(W_in[l], j)
        tl = ntiles(nw)
        def mm(e, t=t):
            for i, (o, w) in enumerate(tl):
                for kc in range(KC):
                    ins = e.matmul(pbank[pbs[i]][:, :w], lhsT=t[:, kc, :], rhs=hT[:, kc, lo + o:lo + o + w], start=(kc == 0), stop=(kc == KC - 1))
            return ins
        P.op("pe", mm, reads=[b, hT_b], writes=[pbuf[pbs[i]] for i in range(len(tl))])
        return tl

    def conv(src, H, n_out, wname, l, g, K, acc):
        wv = V(wname + str(l))
        P_ = []
        for k in range(K - 1, -1, -1):
            sh = K - 1 - k
            o0 = max(0, sh - H)
            if o0 >= n_out:
                continue
            wcol = wv[:, k * GC + g:k * GC + g + 1]
            if k == K - 1:
                P_.append(lambda e, wcol=wcol: e.tensor_scalar(out=acc[:, :n_out], in0=src[:, H:H + n_out], scalar1=wcol, scalar2=None, op0=ALU.mult))
            else:
                P_.append(lambda e, wcol=wcol, o0=o0, sh=sh: e.scalar_tensor_tensor(out=acc[:, o0:n_out], in0=src[:, H + o0 - sh:H + n_out - sh], scalar=wcol,
                                                                               in1=acc[:, o0:n_out], op0=ALU.mult, op1=ALU.add))
        return P_

    def mixer(l, c0, n_out, H, mask):
        nw = n_out + H
        norm_mod(l, 0, c0 - H, nw)
        pend_stats = []
        for g in range(GC):
            tl = inproj(l, GC + g, nw, [0, 1])
            zc, zcb = zc_p.get()
            for i, (o, w) in enumerate(tl):
                P.op("act", lambda e, i=i, o=o, w=w, zc=zc: e.activation(out=zc[:, o:o + w], in_=pbank[i][:, :w], func=AF.Copy), reads=[pbuf[i]], writes=[zcb])
            tl = inproj(l, 2 * GC + g, nw, [2, 3])
            cv, cvb = tmpp.get()
            for i, (o, w) in enumerate(tl):
                P.op("dve", lambda e, i=i, o=o, w=w, zc=zc, cv=cv: e.tensor_tensor(out=cv[:, o:o + w], in0=pbank[2 + i][:, :w], in1=zc[:, o:o + w], op=ALU.mult),
                     reads=[pbuf[2 + i], zcb], writes=[cvb])
            if mask:
                P.op("dve", lambda e, cv=cv: e.tensor_scalar(out=cv[:, 0:H], in0=cv[:, 0:H], scalar1=V("hmask"), scalar2=None, op0=ALU.mult), reads=[cvb, cb], writes=[cvb])
            acc, accb = acc_p.get()
            for f in conv(cv, H, n_out, "scw", l, g, 3, acc):
                P.op("dve", f, reads=[cvb, cb], writes=[accb])
            tl = inproj(l, g, n_out, [0], lo=H)
            P.op("dve", lambda e, acc=acc, g=g: e.tensor_tensor(out=yT[:, g, :n_out], in0=pbank[0][:, :n_out], in1=acc[:, :n_out], op=ALU.mult),
                 reads=[pbuf[0], accb], writes=[yT_b])
            tl = inproj(l, 4 * GC + g, nw, [2, 3])
            sg, sgb = zc_p.get()
            for i, (o, w) in enumerate(tl):
                P.op("act", lambda e, i=i, o=o, w=w, sg=sg: e.activation(out=sg[:, o:o + w], in_=pbank[2 + i][:, :w], func=AF.Sigmoid), reads=[pbuf[2 + i]], writes=[sgb])
            tl = inproj(l, 3 * GC + g, nw, [0, 1])
            u, ub = tmpp.get()
            for i, (o, w) in enumerate(tl):
                P.op("dve", lambda e, i=i, o=o, w=w, sg=sg, u=u: e.tensor_tensor(out=u[:, o:o + w], in0=pbank[i][:, :w], in1=sg[:, o:o + w], op=ALU.mult),
                     reads=[pbuf[i], sgb], writes=[ub])
            if mask:
                P.op("dve", lambda e, u=u: e.tensor_scalar(out=u[:, 0:H], in0=u[:, 0:H], scalar1=V("hmask"), scalar2=None, op0=ALU.mult), reads=[ub, cb], writes=[ub])
            acc, accb = acc_p.get()
            for f in conv(u, H, n_out, "cfw", l, g, 31, acc):
                P.op("dve", f, reads=[ub, cb], writes=[accb])
            P.op("act", lambda e, acc=acc, g=g: e.activation(out=u2T[:, g, :n_out], in_=acc[:, :n_out], func=AF.Identity, bias=V(f"cfb{l}", g), scale=1.0),
                 reads=[accb, cb], writes=[u2_b[g]])
            sq, sqb = tmpp.get()
            P.op("act", lambda e, sq=sq, g=g: e.activation(out=sq[:, :n_out], in_=u2T[:, g, :n_out], func=AF.Square), reads=[u2_b[g]], writes=[sqb])
            def mmst(e, g=g, sq=sq):
                e.matmul(pbank[4][:, :n_out], lhsT=ones[:], rhs=u2T[:, g, :n_out], start=(g == 0), stop=(g == GC - 1))
                return e.matmul(pbank[5][:, :n_out], lhsT=ones[:], rhs=sq[:, :n_out], start=(g == 0), stop=(g == GC - 1))
            pend_stats.append((mmst, [u2_b[g], sqb, cb]))
            if len(pend_stats) > 1:
                f_, r_ = pend_stats.pop(0)
                P.op("pe", f_, reads=r_, writes=[pbuf[4], pbuf[5]])
        for f_, r_ in pend_stats:
            P.op("pe", f_, reads=r_, writes=[pbuf[4], pbuf[5]])
        DCF = D // 2
        P.op("act", lambda e: e.activation(out=mu[:, :n_out], in_=pbank[4][:, :n_out], func=AF.Copy, scale=1.0 / DCF), reads=[pbuf[4]], writes=[mu_b])
        m2, m2b = tmpp.get()
        P.op("dve", lambda e, m2=m2: e.tensor_tensor(out=m2[:, :n_out], in0=mu[:, :n_out], in1=mu[:, :n_out], op=ALU.mult), reads=[mu_b], writes=[m2b])
        P.op("dve", lambda e, m2=m2: e.scalar_tensor_tensor(out=m2[:, :n_out], in0=pbank[5][:, :n_out], scalar=1.0 / DCF, in1=m2[:, :n_out], op0=ALU.mult, op1=ALU.subtract),
             reads=[pbuf[5], m2b], writes=[m2b])
        P.op("act", lambda e, m2=m2: e.activation(out=rstd[:, :n_out], in_=m2[:, :n_out], func=AF.Sqrt, bias=epsl[:], scale=1.0), reads=[m2b, cb], writes=[rstd_b])
        P.op("dve", lambda e: e.reciprocal(out=rstd[:, :n_out], in_=rstd[:, :n_out]), reads=[rstd_b], writes=[rstd_b])
        for g in range(GC):
            t1, t1b = tmpp.get()
            P.op("dve", lambda e, t1=t1, g=g: e.tensor_tensor(out=t1[:, :n_out], in0=u2T[:, g, :n_out], in1=mu[:, :n_out], op=ALU.subtract), reads=[u2_b[g], mu_b], writes=[t1b])
            P.op("dve", lambda e, t1=t1: e.tensor_tensor(out=t1[:, :n_out], in0=t1[:, :n_out], in1=rstd[:, :n_out], op=ALU.mult), reads=[t1b, rstd_b], writes=[t1b])
            P.op("act", lambda e, t1=t1, g=g: e.activation(out=yT[:, GC + g, :n_out], in_=t1[:, :n_out], func=AF.Silu, scale=V(f"cfg{l}", g), bias=V(f"cfbeta{l}", g)),
                 reads=[t1b, cb], writes=[yT_b])
        pre = resid_load(0, c0, n_out)
        for dc in range(KC):
            t, b = load_w(W_out[l], dc)
            pb = 6 + dc % 2
            def mm(e, t=t, pb=pb):
                for kc in range(KC):
                    ins = e.matmul(pbank[pb][:, :n_out], lhsT=t[:, kc, :], rhs=yT[:, kc, :n_out], start=(kc == 0), stop=(kc == KC - 1))
                return ins
            P.op("pe", mm, reads=[b, yT_b], writes=[pbuf[pb]])
            nxt = resid_load(dc + 1, c0, n_out) if dc + 1 < KC else None
            resid_fin(l, 2, dc, c0, n_out, pb, None, pre)
            pre = nxt

    def resid_load(dc, c0, n):
        xc, xb = xcp.get()
        P.op("act", lambda e: e.dma_start(out=xc[:, :n], in_=xT[dc, :, c0:c0 + n]), reads=xT_bufs(c0, n), writes=[xb], dma=True)
        return xc, xb

    def resid_fin(l, gi, dc, c0, n, pb, gmask, pre):
        xc, xb = pre
        gcol = mods[l][:, gi * KC + dc:gi * KC + dc + 1]
        if gmask is None:
            P.op("dve", lambda e: e.scalar_tensor_tensor(out=xc[:, :n], in0=pbank[pb][:, :n], scalar=gcol, in1=xc[:, :n], op0=ALU.mult, op1=ALU.add),
                 reads=[pbuf[pb], xb, mod_b[l]], writes=[xb])
        else:
            tm, tmb = tmpp.get()
            P.op("dve", lambda e: e.tensor_tensor(out=tm[:, :n], in0=pbank[pb][:, :n], in1=gmask, op=ALU.mult), reads=[pbuf[pb], Gbc_b], writes=[tmb])
            P.op("dve", lambda e: e.scalar_tensor_tensor(out=xc[:, :n], in0=tm[:, :n], scalar=gcol, in1=xc[:, :n], op0=ALU.mult, op1=ALU.add),
                 reads=[tmb, xb, mod_b[l]], writes=[xb])
        P.op("act", lambda e: e.dma_start(out=xT[dc, :, c0:c0 + n], in_=xc[:, :n]), reads=[xb], writes=xT_bufs(c0, n), dma=True)

    sa_p = Pool_(2, [128, BLK], F32, "sa")
    def ffn_part(l, c0, n, wg, wu, wd, f0, f1, gmask):
        nf = f1 - f0
        for fc in range(f0, f1):
            tg, bg = load_w(wg, fc)
            tu, bu = load_w(wu, fc)
            def mm(e, tg=tg, tu=tu):
                for kc in range(KC):
                    e.matmul(pbank[0][:, :n], lhsT=tg[:, kc, :], rhs=hT[:, kc, :n], start=(kc == 0), stop=(kc == KC - 1))
                for kc in range(KC):
                    ins = e.matmul(pbank[1][:, :n], lhsT=tu[:, kc, :], rhs=hT[:, kc, :n], start=(kc == 0), stop=(kc == KC - 1))
                return ins
            pa, pu = (0, 1) if (fc - f0) % 2 == 0 else (2, 3)
            def mm2(e, tg=tg, tu=tu, pa=pa, pu=pu):
                for kc in range(KC):
                    e.matmul(pbank[pa][:, :n], lhsT=tg[:, kc, :], rhs=hT[:, kc, :n], start=(kc == 0), stop=(kc == KC - 1))
                for kc in range(KC):
                    ins = e.matmul(pbank[pu][:, :n], lhsT=tu[:, kc, :], rhs=hT[:, kc, :n], start=(kc == 0), stop=(kc == KC - 1))
                return ins
            P.op("pe", mm2, reads=[bg, bu, hT_b], writes=[pbuf[pa], pbuf[pu]])
            sa, sab = sa_p.get()
            P.op("act", lambda e, sa=sa, pa=pa: e.activation(out=sa[:, :n], in_=pbank[pa][:, :n], func=AF.Silu), reads=[pbuf[pa]], writes=[sab])
            P.op("dve", lambda e, sa=sa, pu=pu, fc=fc: e.tensor_tensor(out=hb[:, fc - f0, :n], in0=pbank[pu][:, :n], in1=sa[:, :n], op=ALU.mult),
                 reads=[pbuf[pu], sab], writes=[hb_b])
        pre = resid_load(0, c0, n)
        for dc in range(KC):
            t, b = wpool.get()
            P.op("sp", lambda e, t=t, dc=dc: e.dma_start(out=t[:, :nf, :], in_=wd.dst[dc][:, f0:f1, :]), reads=[wd.bufs[dc]], writes=[b], dma=True)
            pb = 6 + dc % 2
            def mm(e, t=t, pb=pb):
                for k in range(nf):
                    ins = e.matmul(pbank[pb][:, :n], lhsT=t[:, k, :], rhs=hb[:, k, :n], start=(k == 0), stop=(k == nf - 1))
                return ins
            P.op("pe", mm, reads=[b, hb_b], writes=[pbuf[pb]])
            nxt = resid_load(dc + 1, c0, n) if dc + 1 < KC else None
            resid_fin(l, 5, dc, c0, n, pb, gmask, pre)
            pre = nxt

    def split(n, parts):
        q, r = divmod(n, parts)
        res, o = [], 0
        for i in range(parts):
            s = q + (1 if i < r else 0)
            if s:
                res.append((o, o + s))
            o += s
        return res

    lg = sb([128, NSUB, NE], F32, "lg"); lg2 = sb([128, NSUB, NE], F32, "lg2")
    mk1 = sb([128, NSUB, NE], F32, "mk1"); mk2 = sb([128, NSUB, NE], F32, "mk2"); G = sb([128, NSUB, NE], F32, "G")
    m1 = sb([128, NSUB], F32, "m1"); m2_ = sb([128, NSUB], F32, "m2"); g1 = sb([128, NSUB], F32, "g1"); g2 = sb([128, NSUB], F32, "g2")
    GT = sb([NE, BLK], F32, "GT")
    rt_b = Buf("router")

    def ffn(l, c0, n):
        moe = (l % 2 == 1)
        norm_mod(l, 1, c0, n, router=moe)
        if not moe:
            for (f0, f1) in split(FC, 3):
                ffn_part(l, c0, n, W_fg, W_fu, W_fd, f0, f1, None)
            return
        for s in range(NSUB):
            P.op("dve", lambda e, s=s: e.tensor_copy(out=lg[:, s, :], in_=pbank[s][:, 0:NE]), reads=[pbuf[s]], writes=[rt_b])
        P.op("dve", lambda e: e.tensor_reduce(out=m1[:], in_=lg[:], axis=AX.X, op=ALU.max), reads=[rt_b], writes=[rt_b])
        for s in range(NSUB):
            P.op("dve", lambda e, s=s: e.tensor_scalar(out=mk1[:, s, :], in0=lg[:, s, :], scalar1=m1[:, s:s + 1], scalar2=None, op0=ALU.is_equal), reads=[rt_b], writes=[rt_b])
        P.op("dve", lambda e: e.scalar_tensor_tensor(out=lg2[:], in0=mk1[:], scalar=-1e30, in1=lg[:], op0=ALU.mult, op1=ALU.add), reads=[rt_b], writes=[rt_b])
        P.op("dve", lambda e: e.tensor_reduce(out=m2_[:], in_=lg2[:], axis=AX.X, op=ALU.max), reads=[rt_b], writes=[rt_b])
        for s in range(NSUB):
            P.op("dve", lambda e, s=s: e.tensor_scalar(out=mk2[:, s, :], in0=lg2[:, s, :], scalar1=m2_[:, s:s + 1], scalar2=None, op0=ALU.is_equal), reads=[rt_b], writes=[rt_b])
        P.op("dve", lambda e: e.tensor_tensor(out=g2[:], in0=m2_[:], in1=m1[:], op=ALU.subtract), reads=[rt_b], writes=[rt_b])
        P.op("act", lambda e: e.activation(out=g2[:], in_=g2[:], func=AF.Exp), reads=[rt_b], writes=[rt_b])
        P.op("dve", lambda e: e.tensor_scalar(out=g1[:], in0=g2[:], scalar1=1.0, scalar2=None, op0=ALU.add), reads=[rt_b], writes=[rt_b])
        P.op("dve", lambda e: e.reciprocal(out=g1[:], in_=g1[:]), reads=[rt_b], writes=[rt_b])
        P.op("dve", lambda e: e.tensor_tensor(out=g2[:], in0=g2[:], in1=g1[:], op=ALU.mult), reads=[rt_b], writes=[rt_b])
        for s in range(NSUB):
            P.op("dve", lambda e, s=s: e.tensor_scalar(out=G[:, s, :], in0=mk1[:, s, :], scalar1=g1[:, s:s + 1], scalar2=None, op0=ALU.mult), reads=[rt_b], writes=[rt_b])
            P.op("dve", lambda e, s=s: e.scalar_tensor_tensor(out=G[:, s, :], in0=mk2[:, s, :], scalar=g2[:, s:s + 1], in1=G[:, s, :], op0=ALU.mult, op1=ALU.add), reads=[rt_b], writes=[rt_b])
        for s in range(NSUB):
            P.op("pe", lambda e, s=s: e.transpose(pbank[4][0:NE, s * 128:(s + 1) * 128], G[:, s, :], ident[:]), reads=[rt_b, cb], writes=[pbuf[4]])
        P.op("act", lambda e: e.activation(out=GT[:], in_=pbank[4][0:NE, :n], func=AF.Copy), reads=[pbuf[4]], writes=[rt_b])
        for ex in range(NE):
            pb = 4 + ex % 2
            P.op("pe", lambda e, ex=ex, pb=pb: e.matmul(pbank[pb][:, :n], lhsT=sel[:, ex * 128:(ex + 1) * 128], rhs=GT[:, :n], start=True, stop=True), reads=[rt_b, cb], writes=[pbuf[pb]])
            P.op("act", lambda e, ex=ex, pb=pb: e.activation(out=Gbc[:, 0, :], in_=pbank[pb][:, :n], func=AF.Copy), reads=[pbuf[pb]], writes=[Gbc_b])
            for (f0, f1) in split(FE, 1):
                ffn_part(l, c0, n, W_mg[ex], W_mu[ex], W_md[ex], f0, f1, Gbc[:, 0, :])

    def final(tt):
        c0, n = tt * 128, 128
        rms_stats(c0, n, [4])
        xn = SCR[:, 0:D].rearrange("p (k c) -> p k c", c=128)
        ot = SCR[:, D:2 * D]
        pend = [xload(kc, c0, n) for kc in range(min(2, KC))]
        for kc in range(KC):
            if kc + 2 < KC:
                pend.append(xload(kc + 2, c0, n))
            xc, xb = pend.pop(0)
            P.op("dve", lambda e, xc=xc, kc=kc: e.scalar_tensor_tensor(out=xn[:, kc, :], in0=xc[:, :n], scalar=V("fg", kc), in1=rstd[:, :n], op0=ALU.mult, op1=ALU.mult),
                 reads=[xb, rstd_b, cb], writes=[scr_b[0]])
        for c4 in range(KC // 4):
            pb = c4 % 4
            def tr(e, c4=c4, pb=pb):
                for q in range(4):
                    ins = e.transpose(pbank[pb][:, q * 128:(q + 1) * 128], xn[:, c4 * 4 + q, :], ident[:])
                return ins
            P.op("pe", tr, reads=[scr_b[0], cb], writes=[pbuf[pb]])
            if c4 % 2 == 0:
                P.op("act", lambda e, c4=c4, pb=pb: e.activation(out=ot[:, c4 * 512:(c4 + 1) * 512], in_=pbank[pb][:], func=AF.Copy), reads=[pbuf[pb]], writes=[scr_b[1]])
            else:
                P.op("dve", lambda e, c4=c4, pb=pb: e.tensor_copy(out=ot[:, c4 * 512:(c4 + 1) * 512], in_=pbank[pb][:]), reads=[pbuf[pb]], writes=[scr_b[1]])
        return [P.op("act", lambda e: e.dma_start(out=out_d[c0 - HALO:c0 - HALO + 128, :], in_=ot), reads=[scr_b[1]], dma=True)]

    main = [(HALO + i * BLK, BLK) for i in range(NB)]
    for l in range(L):
        for i in reversed(range(NB)):
            c0, n = main[i]
            mixer(l, c0, n, HALO, mask=(i == 0))
        if l + 1 < L:
            mixer(l, 0, HALO, 0, mask=False)
            ffn(l, 0, HALO)
        for (c0, n) in main:
            ffn(l, c0, n)
    alltoks = []
    for tt in range(1, T // 128):
        alltoks += final(tt)
    P.final_wait("act", alltoks[-12:])
    P.emit_all()
    P.close()
    es.close()
    return nc


def _pack_inputs(cfg, b, inp, hm):
    KC, GC, NE, L = cfg.KC, cfg.GC, cfg.NE, cfg.L
    vecs = np.zeros((128, cfg.NV), np.float32)
    def put(name, arr):
        o, n = cfg.off[name]
        a = np.asarray(arr, np.float32)
        if a.ndim == 1:
            vecs[:, o:o + n] = a.reshape(-1, 128).T
        else:
            k = a.shape[0]
            vecs[:, o:o + n] = a.reshape(k, -1, 128).transpose(2, 0, 1).reshape(128, -1)
    for l in range(L):
        put(f"gmix{l}", inp["norm_mix_g"][l]); put(f"gffn{l}", inp["norm_ffn_g"][l]); put(f"bada{l}", inp["b_ada"][l])
        put(f"scw{l}", inp["sc_conv_w"][l]); put(f"cfw{l}", inp["cf_conv_w"][l])
        put(f"cfb{l}", inp["cf_conv_b"][l]); put(f"cfg{l}", inp["cf_ln_g"][l]); put(f"cfbeta{l}", inp["cf_ln_b"][l])
    put("fg", inp["final_g"]); put("c", inp["c"][b])
    o, n = cfg.off["hmask"]
    vecs[:, o] = hm
    o, n = cfg.off["rw"]
    rw = np.asarray(inp["router_w"][0], np.float32)
    vecs[:, o:o + n] = rw.reshape(KC, 128, NE).transpose(1, 0, 2).reshape(128, KC * NE)
    return vecs


_CACHE = {}


def run(cfg, inputs):
    key = (cfg.D, cfg.DFF, cfg.DFE, cfg.S, cfg.BLK, cfg.NCPB)
    if key not in _CACHE:
        _CACHE[key] = build(cfg)
    nc = _CACHE[key]
    NE = cfg.NE
    sel = np.zeros((NE, NE * 128), np.float32)
    for e in range(NE):
        sel[e, e * 128:(e + 1) * 128] = 1.0
    B = inputs["x"].shape[0]
    in_maps = []
    f = lambda a: np.ascontiguousarray(np.asarray(a, np.float32))
    shared = dict(
        ident=np.eye(128, dtype=np.float32), ones=np.ones((128, 128), np.float32), sel=sel,
        w_ada=f(inputs["w_ada"]), w_in=f(inputs["w_in"]), w_out=f(inputs["w_out"]),
        ffn_w_gate=f(inputs["ffn_w_gate"][0]), ffn_w_up=f(inputs["ffn_w_up"][0]), ffn_w_down=f(inputs["ffn_w_down"][0]),
        moe_w_gate=f(inputs["moe_w_gate"][0]), moe_w_up=f(inputs["moe_w_up"][0]), moe_w_down=f(inputs["moe_w_down"][0]),
    )
    NCPB, TM, T = cfg.NCPB, cfg.TM, cfg.T
    x = np.asarray(inputs["x"], np.float32)
    for b in range(B):
        for j in range(NCPB):
            m = dict(shared)
            xs = np.zeros((T, cfg.D), np.float32)
            lo = j * TM
            if j > 0:
                xs[:] = x[b, lo - 128:lo + TM]
            else:
                xs[128:] = x[b, 0:TM]
            m["x"] = xs
            m["vecs"] = _pack_inputs(cfg, b, inputs, 0.0 if j == 0 else 1.0)
            in_maps.append(m)
    ncores = B * NCPB
    res = run_bass_kernel_spmd(nc, in_maps, core_ids=list(range(ncores)))
    out = np.empty((B, cfg.S, cfg.D), np.float32)
    for b in range(B):
        for j in range(NCPB):
            out[b, j * TM:(j + 1) * TM] = res.results[b * NCPB + j]["out"]
    return out


def kernel(**inputs):
    cfg = Cfg()
    return run(cfg, inputs)
```
